# Optimizing a Trainium2 kernel written in Bass

```python
import jax, jax.numpy as jnp
from jax import lax
import numpy as np

D_MODEL = 1024
BATCH = 8
SEQ = 4096
DEPTH = 2

HEAD_DIM = 64
ATTN_GROUPS = ((128, 1), (512, 4), (2048, 16))
N_GROUPS = 3
HEADS_PER_GROUP = 4
ATTN_WIDTH = N_GROUPS * HEADS_PER_GROUP * HEAD_DIM
ATTN_OUT = HEADS_PER_GROUP * HEAD_DIM
WIN_BLOCK = 128
ROPE_THETA = 10000.0

GMLP_CHUNK = 128
GMLP_GROUPS = 4
GMLP_GROUP_DIM = 128
GMLP_WIDTH = GMLP_GROUPS * GMLP_GROUP_DIM

N_EXPERTS = 32
TOP_K = 4
D_FF = D_MODEL
SWIGLU_ALPHA = 1.702
SWIGLU_LIMIT = 7.0
MOE_BLOCK = 128

N_BRANCH = 2
IN_WIDTH = 3 * ATTN_WIDTH + 2 * GMLP_WIDTH + N_BRANCH * D_MODEL
RMS_EPS = 1e-5
LN_EPS = 1e-5
NEG_INF = -1e30

kernel_name = "hybrid_dilated_attn_gmlp_moe_block"


def rmsnorm(x, g):
    xf = x.astype(jnp.float32)
    y = xf * lax.rsqrt(jnp.mean(xf * xf, axis=-1, keepdims=True) + RMS_EPS)
    return (y * g.astype(jnp.float32)).astype(x.dtype)


def layernorm(x, g, b):
    xf = x.astype(jnp.float32)
    mu = jnp.mean(xf, axis=-1, keepdims=True)
    var = jnp.mean(jnp.square(xf - mu), axis=-1, keepdims=True)
    y = (xf - mu) * lax.rsqrt(var + LN_EPS)
    return (y * g.astype(jnp.float32) + b.astype(jnp.float32)).astype(x.dtype)


def rope(x, positions):
    half = HEAD_DIM // 2
    inv_freq = ROPE_THETA ** (-jnp.arange(half, dtype=jnp.float32) / half)
    ang = positions.astype(jnp.float32)[..., None] * inv_freq
    cos = jnp.cos(ang)[:, :, None, :]
    sin = jnp.sin(ang)[:, :, None, :]
    xf = x.astype(jnp.float32)
    x1, x2 = xf[..., :half], xf[..., half:]
    out = jnp.concatenate([x1 * cos - x2 * sin, x2 * cos + x1 * sin], axis=-1)
    return out.astype(x.dtype)


def dilated_window_attention(q, k, v, window, dilation):
    B, S, H, Dh = q.shape
    r = dilation
    L = S // r
    wn = window // r
    nb = -(-L // WIN_BLOCK)
    Lp = nb * WIN_BLOCK

    def to_sub(t):
        t = t.reshape(B, L, r, H, Dh).transpose(0, 2, 1, 3, 4).reshape(B * r, L, H, Dh)
        t = jnp.pad(t, ((0, 0), (0, Lp - L), (0, 0), (0, 0)))
        return t.reshape(B * r, nb, WIN_BLOCK, H, Dh)

    qs, ks, vs = to_sub(q), to_sub(k), to_sub(v)

    def with_prev(t):
        prev = jnp.pad(t, ((0, 0), (1, 0), (0, 0), (0, 0), (0, 0)))[:, :-1]
        return jnp.concatenate([prev, t], axis=2)

    kb, vb = with_prev(ks), with_prev(vs)
    s = jnp.einsum('znqhd,znkhd->znhqk', qs.astype(jnp.float32), kb.astype(jnp.float32))
    s = s * (Dh ** -0.5)
    qi = jnp.arange(WIN_BLOCK)[:, None]
    kj = jnp.arange(2 * WIN_BLOCK)[None, :]
    dist = WIN_BLOCK + qi - kj
    band = (dist >= 0) & (dist <= wn)
    key_pos = jnp.arange(nb)[:, None, None] * WIN_BLOCK - WIN_BLOCK + kj
    valid = band[None] & (key_pos >= 0)
    s = jnp.where(valid[None, :, None], s, NEG_INF)
    m = jnp.max(s, axis=-1, keepdims=True)
    p = jnp.exp(s - m)
    den = jnp.sum(p, axis=-1, keepdims=True)
    o = jnp.einsum('znhqk,znkhd->znqhd', p, vb.astype(jnp.float32))
    o = o / den.transpose(0, 1, 3, 2, 4)
    lse = (m + jnp.log(den)).transpose(0, 1, 3, 2, 4)

    def from_sub(t):
        t = t.reshape(B, r, Lp, H, t.shape[-1])[:, :, :L]
        return t.transpose(0, 2, 1, 3, 4).reshape(B, S, H, t.shape[-1])

    return from_sub(o), from_sub(lse)


def token_mixers(h, positions, w_in, w_s, b_s, ln_g, ln_b, w_pa, w_pg, w_o):
    B, S, _ = h.shape
    proj = h @ w_in
    cuts = [ATTN_WIDTH, 2 * ATTN_WIDTH, 3 * ATTN_WIDTH,
            3 * ATTN_WIDTH + GMLP_WIDTH, 3 * ATTN_WIDTH + 2 * GMLP_WIDTH]
    q, k, v, u, vg, gates = jnp.split(proj, cuts, axis=-1)

    n_heads = N_GROUPS * HEADS_PER_GROUP
    grp = (B, S, N_GROUPS, HEADS_PER_GROUP, HEAD_DIM)
    q = rope(q.reshape(B, S, n_heads, HEAD_DIM), positions).reshape(grp)
    k = rope(k.reshape(B, S, n_heads, HEAD_DIM), positions).reshape(grp)
    v = v.reshape(grp)
    outs, lses = [], []
    for g, (win, dil) in enumerate(ATTN_GROUPS):
        o_g, l_g = dilated_window_attention(q[:, :, g], k[:, :, g], v[:, :, g], win, dil)
        outs.append(o_g)
        lses.append(l_g)
    o_all = jnp.stack(outs, axis=0)
    w_den = jax.nn.softmax(jnp.stack(lses, axis=0), axis=0)
    attn = jnp.sum(w_den * o_all, axis=0).reshape(B, S, ATTN_OUT).astype(h.dtype)

    u = jax.nn.gelu(u)
    vg = layernorm(jax.nn.gelu(vg), ln_g, ln_b)
    nc = S // GMLP_CHUNK
    vc = vg.reshape(B, nc, GMLP_CHUNK, GMLP_GROUPS, GMLP_GROUP_DIM)
    tri = jnp.tril(jnp.ones((GMLP_CHUNK, GMLP_CHUNK), dtype=bool))
    ws = jnp.where(tri[None], w_s, jnp.zeros_like(w_s))
    mixed = jnp.einsum('gts,bnsgc->bntgc', ws, vc) + b_s.T[None, None, :, :, None]
    gm = u * mixed.reshape(B, S, GMLP_WIDTH)

    g_a, g_g = jnp.split(gates, N_BRANCH, axis=-1)
    merged = jax.nn.sigmoid(g_a) * (attn @ w_pa) + jax.nn.sigmoid(g_g) * (gm @ w_pg)
    return merged @ w_o


def clamped_swiglu(hb):
    x_glu = jnp.minimum(hb[..., ::2], SWIGLU_LIMIT)
    x_lin = jnp.clip(hb[..., 1::2], -SWIGLU_LIMIT, SWIGLU_LIMIT)
    return x_glu * jax.nn.sigmoid(SWIGLU_ALPHA * x_glu) * (x_lin + 1)


def moe(h, w_router, b_router, w1, b1, w2, b2):
    B, S, D = h.shape
    N = B * S
    xt = h.reshape(N, D)
    logits = (xt @ w_router + b_router).astype(jnp.float32)
    top_vals, top_idx = lax.top_k(logits, TOP_K)
    gate = jax.nn.softmax(top_vals, axis=-1)

    A = N * TOP_K
    e_flat = top_idx.reshape(A)
    tok_flat = jnp.arange(A, dtype=jnp.int32) // TOP_K
    order = jnp.argsort(e_flat)
    e_sorted = e_flat[order]
    tok_sorted = tok_flat[order]
    gate_sorted = gate.reshape(A)[order]

    counts = jnp.zeros((N_EXPERTS,), jnp.int32).at[e_flat].add(1)
    start = jnp.cumsum(counts) - counts
    padded = (counts + MOE_BLOCK - 1) // MOE_BLOCK * MOE_BLOCK
    pad_end = jnp.cumsum(padded)
    pad_start = pad_end - padded
    rank = jnp.arange(A, dtype=jnp.int32) - start[e_sorted]
    dest = pad_start[e_sorted] + rank

    n_blocks = -(-A // MOE_BLOCK) + N_EXPERTS
    P = n_blocks * MOE_BLOCK
    buf = jnp.zeros((P, D), h.dtype).at[dest].set(xt[tok_sorted])
    block_start = jnp.arange(n_blocks, dtype=jnp.int32) * MOE_BLOCK
    block_e = jnp.minimum(jnp.searchsorted(pad_end, block_start, side='right'),
                          N_EXPERTS - 1).astype(jnp.int32)

    def expert_block(args):
        xb, e = args
        hb = xb @ w1[e] + b1[e]
        return clamped_swiglu(hb) @ w2[e] + b2[e]

    out = lax.map(expert_block, (buf.reshape(n_blocks, MOE_BLOCK, D), block_e))
    y_assign = out.reshape(P, D)[dest] * gate_sorted[:, None].astype(out.dtype)
    y = jax.ops.segment_sum(y_assign, tok_sorted, num_segments=N)
    return y.reshape(B, S, D).astype(h.dtype)


def setup_inputs(seed: int = 0) -> dict:
    key = jax.random.key(seed)
    ks = jax.random.split(key, 24)

    def nrm(k, shape, scale):
        return jax.random.normal(k, shape, jnp.float32) * scale

    x = nrm(ks[0], (BATCH, SEQ, D_MODEL), 1.0)
    c = nrm(ks[1], (BATCH, D_MODEL), 1.0)
    positions = (jax.random.randint(ks[2], (BATCH, 1), 0, 1024, dtype=jnp.int32)
                 + jnp.arange(SEQ, dtype=jnp.int32)[None, :])
    return {
        "x": x,
        "c": c,
        "positions": positions,
        "w_ada": nrm(ks[3], (DEPTH, D_MODEL, 6 * D_MODEL), 0.5 * D_MODEL ** -0.5),
        "b_ada": nrm(ks[4], (DEPTH, 6 * D_MODEL), 0.02),
        "norm1_g": 1.0 + nrm(ks[5], (DEPTH, D_MODEL), 0.02),
        "w_in": nrm(ks[6], (DEPTH, D_MODEL, IN_WIDTH), D_MODEL ** -0.5),
        "w_s": nrm(ks[7], (DEPTH, GMLP_GROUPS, GMLP_CHUNK, GMLP_CHUNK), GMLP_CHUNK ** -0.5),
        "b_s": 1.0 + nrm(ks[8], (DEPTH, GMLP_GROUPS, GMLP_CHUNK), 0.1),
        "ln_g": 1.0 + nrm(ks[9], (DEPTH, GMLP_WIDTH), 0.02),
        "ln_b": nrm(ks[10], (DEPTH, GMLP_WIDTH), 0.02),
        "w_pa": nrm(ks[11], (DEPTH, ATTN_OUT, D_MODEL), ATTN_OUT ** -0.5),
        "w_pg": nrm(ks[12], (DEPTH, GMLP_WIDTH, D_MODEL), GMLP_WIDTH ** -0.5),
        "w_o": nrm(ks[13], (DEPTH, D_MODEL, D_MODEL), D_MODEL ** -0.5),
        "norm2_g": 1.0 + nrm(ks[14], (DEPTH, D_MODEL), 0.02),
        "w_router": nrm(ks[15], (DEPTH, D_MODEL, N_EXPERTS), D_MODEL ** -0.5),
        "b_router": nrm(ks[16], (DEPTH, N_EXPERTS), 0.01),
        "w1": nrm(ks[17], (DEPTH, N_EXPERTS, D_MODEL, 2 * D_FF), D_MODEL ** -0.5),
        "b1": nrm(ks[18], (DEPTH, N_EXPERTS, 2 * D_FF), 0.01),
        "w2": nrm(ks[19], (DEPTH, N_EXPERTS, D_FF, D_MODEL), D_FF ** -0.5),
        "b2": nrm(ks[20], (DEPTH, N_EXPERTS, D_MODEL), 0.01),
        "final_g": 1.0 + nrm(ks[21], (D_MODEL,), 0.02),
    }


def reference(x, c, positions, w_ada, b_ada, norm1_g, w_in, w_s, b_s, ln_g, ln_b,
              w_pa, w_pg, w_o, norm2_g, w_router, b_router, w1, b1, w2, b2, final_g):
    h = x
    for l in range(DEPTH):
        mod = jax.nn.silu(c) @ w_ada[l] + b_ada[l]
        sh1, sc1, g1, sh2, sc2, g2 = jnp.split(mod[:, None, :], 6, axis=-1)
        a = rmsnorm(h, norm1_g[l]) * (1 + sc1) + sh1
        h = h + g1 * token_mixers(a, positions, w_in[l], w_s[l], b_s[l], ln_g[l], ln_b[l],
                                  w_pa[l], w_pg[l], w_o[l])
        f = rmsnorm(h, norm2_g[l]) * (1 + sc2) + sh2
        h = h + g2 * moe(f, w_router[l], b_router[l], w1[l], b1[l], w2[l], b2[l])
    return rmsnorm(h, final_g)
```

```python
import math
from contextlib import ExitStack

import numpy as np
import concourse.bass as bass
import concourse.mybir as mybir
from concourse.bass_utils import run_bass_kernel_spmd

F32 = mybir.dt.float32
BF16 = mybir.dt.bfloat16
I32 = mybir.dt.int32
ALU = mybir.AluOpType
AF = mybir.ActivationFunctionType
AX = mybir.AxisListType

D = 1024
S = 4096
NCORES = 8
DEPTH = 2
KC = 8
ST = 512
NST = S // ST
E = 32
INW = 5376
RMS_EPS = 1e-5
LN_EPS = 1e-5
GROUPS = ((128, 1), (512, 4), (2048, 16))
TWO_PI = 2.0 * math.pi


class Buf:
    __slots__ = ("name", "w", "r")

    def __init__(self, name):
        self.name = name
        self.w = None
        self.r = []


class Op:
    __slots__ = ("eng", "fn", "deps", "needed", "cnt", "is_dma", "sem", "semval")


class KB:
    ENGS = ("pe", "act", "dve", "pool", "sp")

    def __init__(self, nc, stack):
        self.nc = nc
        self.stack = stack
        self.ops = {e: [] for e in self.ENGS}
        self.last_c = {e: None for e in self.ENGS}
        self.dmas = []
        self.dma_sems = {}
        self.free_sems = {"sw": [], "hw": []}
        self.esem = {}
        for e in self.ENGS:
            self.esem[e] = stack.enter_context(nc.semaphore("es_" + e))
        self.nsem = 5

    def _mk(self, eng, fn, reads, writes, is_dma):
        op = Op()
        op.eng = eng
        op.fn = fn
        op.needed = False
        op.cnt = 0
        op.is_dma = is_dma
        op.sem = None
        op.semval = 0
        deps = []
        for b in reads:
            if b.w is not None:
                deps.append(b.w)
        for b in writes:
            if b.w is not None:
                deps.append(b.w)
            deps.extend(b.r)
        seen = set()
        out = []
        for d in deps:
            if id(d) in seen:
                continue
            seen.add(id(d))
            if (not d.is_dma) and (not is_dma) and d.eng == "pe" and eng == "pe":
                continue
            out.append(d)
        op.deps = out
        for b in reads:
            if not is_dma:
                b.r = [x for x in b.r if x.is_dma or x.eng != eng]
            b.r.append(op)
        for b in writes:
            b.w = op
            b.r = []
        self.ops[eng].append(op)
        if is_dma:
            self.dmas.append(op)
        else:
            self.last_c[eng] = op
        return op

    def op(self, eng, fn, reads=(), writes=()):
        return self._mk(eng, fn, reads, writes, False)

    def dma(self, q, out, in_, sb, reads=(), writes=()):
        op = self._mk(q, lambda e: e.dma_start(out=out, in_=in_), reads, writes, True)
        qt = "sw" if q == "pool" else "hw"
        ent = self.dma_sems.get((id(sb), qt))
        if ent is None:
            if self.free_sems[qt]:
                ent = self.free_sems[qt].pop()
            else:
                sem = self.stack.enter_context(self.nc.semaphore("ds%d" % self.nsem))
                self.nsem += 1
                ent = [sem, 0, qt]
            self.dma_sems[(id(sb), qt)] = ent
        ent[1] += 16
        op.sem = ent[0]
        op.semval = ent[1]
        return op

    def barrier(self):
        lasts = [self.last_c[e] for e in self.ENGS if self.last_c[e] is not None]
        dmas = self.dmas
        self.dmas = []
        for ent in self.dma_sems.values():
            self.free_sems[ent[2]].append(ent)
        self.dma_sems = {}
        for e in self.ENGS:
            op = Op()
            op.eng = e
            op.fn = None
            op.needed = False
            op.cnt = 0
            op.is_dma = False
            op.sem = None
            op.semval = 0
            op.deps = [d for d in lasts + dmas if d.is_dma or d.eng != e]
            self.ops[e].append(op)

    def emit(self):
        nc = self.nc
        for e in self.ENGS:
            for op in self.ops[e]:
                for d in op.deps:
                    d.needed = True
        for e in self.ENGS:
            c = 0
            for op in self.ops[e]:
                if (not op.is_dma) and op.fn is not None and op.needed:
                    c += 1
                op.cnt = c
        esem = self.esem
        allops = self.ops

        def run(ename, eng):
            known = {}
            for op in allops[ename]:
                for d in op.deps:
                    if d.is_dma:
                        sem, val = d.sem, d.semval
                    else:
                        sem, val = esem[d.eng], d.cnt
                    if known.get(id(sem), 0) < val:
                        eng.wait_ge(sem, val)
                        known[id(sem)] = val
                if op.fn is None:
                    continue
                ins = op.fn(eng)
                if op.is_dma:
                    ins.then_inc(op.sem, 16)
                elif op.needed:
                    ins.then_inc(esem[ename], 1)

        with nc.Block() as block:
            @block.tensor
            def _(e):
                run("pe", e)

            @block.scalar
            def _(e):
                run("act", e)

            @block.vector
            def _(e):
                run("dve", e)

            @block.gpsimd
            def _(e):
                run("pool", e)

            @block.sync
            def _(e):
                run("sp", e)


class StopBuild(Exception):
    pass


class T:
    def __init__(self, h, name):
        self.h = h
        self.b = Buf(name)

    def __getitem__(self, k):
        return self.h[k]


def build_nc(cfg=None):
    cfg = cfg or {}
    n_layers = cfg.get("layers", DEPTH)
    do_moe = cfg.get("moe", True)
    n_exp = cfg.get("n_exp", E)
    tap = cfg.get("tap", None)

    nc = bass.Bass("TRN2", target_bir_lowering=False)

    def din(name, shape, dt=F32):
        return nc.dram_tensor(name, list(shape), dt, kind="ExternalInput").ap()

    def dscr(name, shape, dt=F32):
        return nc.dram_tensor(name, list(shape), dt, kind="Internal").ap()

    xT = din("xT", [D, S])
    cT = din("cT", [128, KC])
    pos = din("pos", [1, S], I32)
    cst = din("cst", [128, 4])
    w_ada = din("w_ada", [DEPTH * D, 6 * D])
    b_adaT = din("b_adaT", [DEPTH * 128, 48])
    n1g = din("n1g", [DEPTH * 128, KC])
    n2g = din("n2g", [DEPTH * 128, KC])
    fg = din("fg", [128, KC])
    w_in = din("w_in", [DEPTH * D, INW])
    w_in_sw = din("w_in_sw", [DEPTH * D, 1536])
    w_sT = din("w_sT", [DEPTH * 128, 512])
    maskT = din("maskT", [128, 512])
    attm = din("attm", [128, 512])
    b_s = din("b_s", [DEPTH, 512])
    ln_g = din("ln_g", [DEPTH, 512])
    ln_b = din("ln_b", [DEPTH, 512])
    w_pa = din("w_pa", [DEPTH * 256, D])
    w_pg = din("w_pg", [DEPTH * 512, D])
    w_o = din("w_o", [DEPTH * D, D])
    w_router = din("w_router", [DEPTH * D, E])
    b_router = din("b_router", [DEPTH, E])
    w1 = din("w1", [DEPTH * E * D if do_moe else 128, 2 * D])
    b1T = din("b1T", [DEPTH * 128, E * 16])
    w2 = din("w2", [DEPTH * E * D if do_moe else 128, D])
    b2 = din("b2", [DEPTH * E, D])
    outT = nc.dram_tensor("outT", [D, S], F32, kind="ExternalOutput").ap()

    hT = dscr("hT", [D, S])
    QT = dscr("QT", [768, S], BF16)
    KT = dscr("KT", [768, S], BF16)
    Vd = dscr("Vd", [S, 768], BF16)
    sigA = dscr("sigA", [D, S])
    part2 = dscr("part2", [D, S])
    cosd = dscr("cosd", [128, S])
    sind = dscr("sind", [128, S])
    dbg = None
    if tap is not None:
        dbg = nc.dram_tensor("dbg", list(cfg["tap_shape"]), F32, kind="ExternalOutput").ap()

    top = ExitStack()
    with top:
        kb = KB(nc, top)

        uniq = [0]

        def sb(stack, name, shape, dt=F32):
            uniq[0] += 1
            name = "%s_%d" % (name, uniq[0])
            return T(stack.enter_context(nc.sbuf_tensor(name, list(shape), dt)), name)

        PS = [T(top.enter_context(nc.psum_tensor("ps%d" % i, [128, 512], F32)), "ps%d" % i) for i in range(8)]

        d_hT = [Buf("hT%d" % i) for i in range(NST)]
        d_QT = Buf("QT")
        d_KT = Buf("KT")
        d_V = Buf("V")
        d_sigA = [Buf("sigA%d" % i) for i in range(NST)]
        d_part2 = [Buf("part2%d" % i) for i in range(NST)]
        d_cos = Buf("cosd")
        d_out = Buf("out")
        d_dbg = Buf("dbg")

        ones_f = sb(top, "ones_f", [128, 128])
        ident = sb(top, "ident", [128, 128])
        cst_t = sb(top, "cst_t", [128, 4])
        eps_t = sb(top, "eps_t", [128, 2])
        modv = sb(top, "modv", [128, 48])
        wv = sb(top, "wv", [128, 16])
        kb.op("dve", lambda e: e.memset(ones_f[:], 1.0), writes=[ones_f.b])
        kb.op("dve", lambda e: e.memset(eps_t[:, 0:1], RMS_EPS), writes=[eps_t.b])
        kb.op("dve", lambda e: e.memset(eps_t[:, 1:2], LN_EPS), writes=[eps_t.b])
        kb.dma("sp", cst_t[:], cst, cst_t.b, writes=[cst_t.b])
        with ExitStack() as ph:
            it = sb(ph, "iota_t", [128, 128], I32)
            kb.op("pool", lambda e: e.iota(it[:], [[1, 128]], base=0, channel_multiplier=-1), writes=[it.b])
            kb.op("dve", lambda e: e.tensor_scalar(ident[:], it[:], 0.0, None, ALU.is_equal), reads=[it.b], writes=[ident.b])
            kb.barrier()

        with ExitStack() as ph:
            posi = sb(ph, "posi", [128, S], I32)
            ang = sb(ph, "ang", [128, S])
            t1 = sb(ph, "rt1", [128, S])
            t2 = sb(ph, "rt2", [128, S])
            ki = sb(ph, "rki", [128, S], I32)
            kb.dma("sp", posi[:], pos.partition_broadcast(128), posi.b, writes=[posi.b])
            kb.op("dve", lambda e: e.tensor_copy(ang[:], posi[:]), reads=[posi.b], writes=[ang.b])
            kb.op("dve", lambda e: e.tensor_scalar(ang[:], ang[:], cst_t[:, 0:1], None, ALU.mult),
                  reads=[ang.b, cst_t.b], writes=[ang.b])
            for which, dst in ((0, sind), (1, cosd)):
                shift = 0.0 if which == 0 else math.pi / 2.0
                kb.op("dve", lambda e, sh=shift: e.tensor_scalar(t1[:], ang[:], sh, None, ALU.add),
                      reads=[ang.b], writes=[t1.b])
                kb.op("dve", lambda e: e.tensor_scalar(t2[:], t1[:], 1.0 / TWO_PI, 0.5, ALU.mult, ALU.add),
                      reads=[t1.b], writes=[t2.b])
                kb.op("dve", lambda e: e.tensor_copy(ki[:], t2[:]), reads=[t2.b], writes=[ki.b])
                kb.op("dve", lambda e: e.tensor_copy(t2[:], ki[:]), reads=[ki.b], writes=[t2.b])
                c_hi = float(np.float32(TWO_PI))
                c_lo = float(TWO_PI - np.float64(np.float32(TWO_PI)))
                kb.op("dve", lambda e: e.scalar_tensor_tensor(t1[:], t2[:], -c_hi, t1[:], ALU.mult, ALU.add),
                      reads=[t1.b, t2.b], writes=[t1.b])
                kb.op("dve", lambda e: e.scalar_tensor_tensor(t1[:], t2[:], -c_lo, t1[:], ALU.mult, ALU.add),
                      reads=[t1.b, t2.b], writes=[t1.b])
                kb.op("dve", lambda e: e.tensor_scalar(t2[:], t1[:], -math.pi, TWO_PI, ALU.is_lt, ALU.mult),
                      reads=[t1.b], writes=[t2.b])
                kb.op("dve", lambda e: e.tensor_tensor(t1[:], t1[:], t2[:], ALU.add), reads=[t1.b, t2.b], writes=[t1.b])
                kb.op("dve", lambda e: e.tensor_scalar(t2[:], t1[:], math.pi, -TWO_PI, ALU.is_gt, ALU.mult),
                      reads=[t1.b], writes=[t2.b])
                kb.op("dve", lambda e: e.tensor_tensor(t1[:], t1[:], t2[:], ALU.add), reads=[t1.b, t2.b], writes=[t1.b])
                kb.op("dve", lambda e: e.tensor_scalar(t1[:], t1[:], 3.1415925, -3.1415925, ALU.min, ALU.max),
                      reads=[t1.b], writes=[t1.b])
                kb.op("act", lambda e: e.activation(t2[:], t1[:], AF.Sin), reads=[t1.b], writes=[t2.b])
                if which == 0:
                    kb.op("dve", lambda e: e.tensor_scalar(t2[:], t2[:], cst_t[:, 1:2], None, ALU.mult),
                          reads=[t2.b, cst_t.b], writes=[t2.b])
                kb.dma("sp", dst, t2[:], t2.b, reads=[t2.b], writes=[d_cos])
            kb.barrier()

        if tap == "cos":
            with ExitStack() as ph:
                tt = sb(ph, "tapt", [128, S])
                kb.dma("sp", tt[:], cosd, tt.b, reads=[d_cos], writes=[tt.b])
                kb.dma("sp", dbg[0:128, :], tt[:], tt.b, reads=[tt.b], writes=[d_dbg])
                kb.dma("sp", tt[:], sind, tt.b, reads=[d_cos, d_dbg], writes=[tt.b])
                kb.dma("sp", dbg[128:256, :], tt[:], tt.b, reads=[tt.b], writes=[d_dbg])
                kb.barrier()

        def rmsnorm_to(ph_tiles, h_t, wv_cols, sh_cols, a_bf, a_f32=None, ps_idx=7, a_off=0):
            sq, rstd = ph_tiles
            ps = PS[ps_idx]
            kb.op("act", lambda e: e.activation(sq[:], h_t[:], AF.Square), reads=[h_t.b], writes=[sq.b])
            for kc in range(KC):
                kb.op("pe", lambda e, kc=kc: e.matmul(ps[:], ones_f[:], sq[:, kc, :], start=(kc == 0), stop=(kc == KC - 1)),
                      reads=[ones_f.b, sq.b], writes=[ps.b])
            kb.op("act", lambda e: e.activation(rstd[:], ps[:], AF.Sqrt, bias=eps_t[:, 0:1], scale=1.0 / D),
                  reads=[ps.b, eps_t.b], writes=[rstd.b])
            kb.op("dve", lambda e: e.reciprocal(rstd[:], rstd[:]), reads=[rstd.b], writes=[rstd.b])
            for kc in range(KC):
                kb.op("dve", lambda e, kc=kc: e.tensor_tensor(sq[:, kc, :], h_t[:, kc, :], rstd[:], ALU.mult),
                      reads=[h_t.b, rstd.b], writes=[sq.b])
                kb.op("act", lambda e, kc=kc: e.activation(a_bf[:, kc, a_off:a_off + ST], sq[:, kc, :], AF.Identity,
                                                            bias=modv[:, sh_cols + kc:sh_cols + kc + 1],
                                                            scale=wv[:, wv_cols + kc:wv_cols + kc + 1]),
                      reads=[sq.b, modv.b, wv.b], writes=[a_bf.b])
                if a_f32 is not None:
                    kb.op("act", lambda e, kc=kc: e.activation(a_f32[:, kc, :], sq[:, kc, :], AF.Identity,
                                                                bias=modv[:, sh_cols + kc:sh_cols + kc + 1],
                                                                scale=wv[:, wv_cols + kc:wv_cols + kc + 1]),
                          reads=[sq.b, modv.b, wv.b], writes=[a_f32.b])

        stopf = [False]

        def one_layer(l):
          if True:
              src_h = xT if l == 0 else hT
              with ExitStack() as ph:
                  ct = sb(ph, "ct", [128, KC])
                  sc = sb(ph, "sc", [128, KC])
                  bad = sb(ph, "bad", [128, 48])
                  ng = sb(ph, "ng", [128, 16])
                  wa = [sb(ph, "wa%d" % i, [128, KC, 512]) for i in range(2)]
                  kb.dma("sp", ct[:], cT, ct.b, writes=[ct.b])
                  kb.dma("sp", bad[:], b_adaT[l * 128:(l + 1) * 128, :], bad.b, writes=[bad.b])
                  kb.dma("sp", ng[:, 0:8], n1g[l * 128:(l + 1) * 128, :], ng.b, writes=[ng.b])
                  kb.dma("sp", ng[:, 8:16], n2g[l * 128:(l + 1) * 128, :], ng.b, writes=[ng.b])
                  kb.op("act", lambda e: e.activation(sc[:], ct[:], AF.Silu), reads=[ct.b], writes=[sc.b])
                  ps = PS[0]
                  wav = w_ada[l * D:(l + 1) * D, :].rearrange("(kc p) n -> p kc n", p=128)
                  for cc in range(12):
                      wt = wa[cc % 2]
                      kb.dma("sp", wt[:], wav[:, :, cc * 512:(cc + 1) * 512], wt.b, writes=[wt.b])
                      for j in range(4):
                          col = cc * 4 + j
                          for kc in range(KC):
                              kb.op("pe", lambda e, wt=wt, j=j, kc=kc, col=col: e.matmul(
                                  ps[:, col:col + 1], wt[:, kc, j * 128:(j + 1) * 128], sc[:, kc:kc + 1],
                                  start=(kc == 0), stop=(kc == KC - 1)),
                                  reads=[wt.b, sc.b], writes=[ps.b])
                  kb.op("dve", lambda e: e.tensor_tensor(modv[:], ps[:, 0:48], bad[:], ALU.add),
                        reads=[ps.b, bad.b], writes=[modv.b])
                  kb.op("dve", lambda e: e.scalar_tensor_tensor(wv[:, 0:8], modv[:, 8:16], 1.0, ng[:, 0:8], ALU.add, ALU.mult),
                        reads=[modv.b, ng.b], writes=[wv.b])
                  kb.op("dve", lambda e: e.scalar_tensor_tensor(wv[:, 8:16], modv[:, 32:40], 1.0, ng[:, 8:16], ALU.add, ALU.mult),
                        reads=[modv.b, ng.b], writes=[wv.b])
                  kb.barrier()

              if tap == "mod" and l == cfg.get("tap_layer", 0):
                  kb.dma("sp", dbg[:, 0:48], modv[:], modv.b, reads=[modv.b], writes=[d_dbg])
                  kb.dma("sp", dbg[:, 48:64], wv[:], wv.b, reads=[wv.b], writes=[d_dbg])
                  kb.barrier()
                  return

              with ExitStack() as ph:
                  win = sb(ph, "win", [128, KC, INW], BF16)
                  wsw = sb(ph, "wsw", [128, KC, 1536], BF16)
                  wpg = sb(ph, "wpg", [128, 4, D], BF16)
                  wst = sb(ph, "wst", [128, 512], BF16)
                  wsf = sb(ph, "wsf", [128, 512])
                  mkf = sb(ph, "mkf", [128, 512])
                  lng = sb(ph, "lng", [128, 512])
                  lnb = sb(ph, "lnb", [128, 512])
                  bsr = sb(ph, "bsr", [1, 512])
                  h_t = [sb(ph, "h_t%d" % i, [128, KC, ST]) for i in range(1)]
                  sq = sb(ph, "sq", [128, KC, ST])
                  rstd = sb(ph, "rstd", [128, ST])
                  a_bf = sb(ph, "a_bf", [128, KC, ST], BF16)
                  cs = [sb(ph, "cs%d" % i, [128, 2, ST]) for i in range(2)]
                  r1 = sb(ph, "r1", [128, ST])
                  r2 = sb(ph, "r2", [128, ST])
                  qo = [sb(ph, "qo%d" % i, [128, ST], BF16) for i in range(2)]
                  vo = [sb(ph, "vo%d" % i, [128, 768], BF16) for i in range(2)]
                  g1t = sb(ph, "g1t", [128, ST])
                  g2t = sb(ph, "g2t", [128, ST])
                  g3t = sb(ph, "g3t", [128, ST])
                  vgn2 = [sb(ph, "vgn%d" % i, [128, 512], BF16) for i in range(2)]
                  st1 = sb(ph, "st1", [128, 4])
                  gmT = sb(ph, "gmT", [128, 4, ST], BF16)
                  so = [sb(ph, "so%d" % i, [128, ST]) for i in range(1)]
                  po = [sb(ph, "po%d" % i, [128, ST]) for i in range(1)]

                  winv = w_in[l * D:(l + 1) * D, :].rearrange("(kc p) n -> p kc n", p=128)
                  wswv = w_in_sw[l * D:(l + 1) * D, :].rearrange("(kc p) n -> p kc n", p=128)
                  for c0 in range(0, INW, 1792):
                      for kc in range(KC):
                          kb.dma("pool", win[:, kc, c0:c0 + 1792], winv[:, kc, c0:c0 + 1792], win.b, writes=[win.b])
                      if c0 == 0:
                          for kc in range(KC):
                              kb.dma("pool", wsw[:, kc, :], wswv[:, kc, :], wsw.b, writes=[wsw.b])
                  wpgv = w_pg[l * 512:(l + 1) * 512, :].rearrange("(kc p) n -> p kc n", p=128)
                  for kc in range(4):
                      kb.dma("pool", wpg[:, kc, :], wpgv[:, kc, :], wpg.b, writes=[wpg.b])
                  kb.dma("sp", wsf[:], w_sT[l * 128:(l + 1) * 128, :], wsf.b, writes=[wsf.b])
                  kb.dma("sp", mkf[:], maskT, mkf.b, writes=[mkf.b])
                  kb.op("dve", lambda e: e.tensor_tensor(wst[:], wsf[:], mkf[:], ALU.mult), reads=[wsf.b, mkf.b], writes=[wst.b])
                  kb.dma("sp", lng[:], ln_g[l:l + 1, :].partition_broadcast(128), lng.b, writes=[lng.b])
                  kb.dma("sp", lnb[:], ln_b[l:l + 1, :].partition_broadcast(128), lnb.b, writes=[lnb.b])
                  kb.dma("sp", bsr[:], b_s[l:l + 1, :], bsr.b, writes=[bsr.b])

                  pscnt = [0]

                  def nps():
                      p = PS[pscnt[0] % 6]
                      pscnt[0] += 1
                      return p

                  def proj_fm(wt, c0, rhs_t):
                      p = nps()
                      for kc in range(KC):
                          kb.op("pe", lambda e, kc=kc: e.matmul(p[:], wt[:, kc, c0:c0 + 128], rhs_t[:, kc, :],
                                                                  start=(kc == 0), stop=(kc == KC - 1)),
                                reads=[wt.b, rhs_t.b], writes=[p.b])
                      return p

                  def gelu_from(p_ap, pbuf, out_ap, obuf, shape_t):
                      a1, a2 = shape_t
                      kb.op("act", lambda e: e.activation(a1, p_ap, AF.Square), reads=[pbuf], writes=[g1t.b])
                      kb.op("dve", lambda e: e.tensor_scalar(a1, a1, 0.044715, 1.0, ALU.mult, ALU.add),
                            reads=[g1t.b], writes=[g1t.b])
                      kb.op("dve", lambda e: e.tensor_tensor(a1, a1, p_ap, ALU.mult), reads=[g1t.b, pbuf], writes=[g1t.b])
                      kb.op("act", lambda e: e.activation(a2, a1, AF.Sigmoid, scale=2.0 * math.sqrt(2.0 / math.pi)),
                            reads=[g1t.b], writes=[g2t.b])
                      kb.op("dve", lambda e: e.tensor_tensor(out_ap, a2, p_ap, ALU.mult), reads=[g2t.b, pbuf], writes=[obuf])

                  def load_a(s_):
                      t0_ = s_ * ST
                      ht_ = h_t[0]
                      c_ = cs[s_ % 2]
                      kb.dma("sp", ht_[:], src_h[:, t0_:t0_ + ST].rearrange("(kc p) t -> p kc t", p=128), ht_.b,
                             reads=[d_hT[s_]], writes=[ht_.b])
                      kb.dma("sp", c_[:, 0, :], cosd[:, t0_:t0_ + ST], c_.b, reads=[d_cos], writes=[c_.b])
                      kb.dma("sp", c_[:, 1, :], sind[:, t0_:t0_ + ST], c_.b, reads=[d_cos], writes=[c_.b])

                  for s in range(NST):
                      t0 = s * ST
                      ht = h_t[0]
                      cst2 = cs[s % 2]
                      if s == 0:
                          load_a(0)
                      rmsnorm_to((sq, rstd), ht, 0, 0, a_bf)
                      if s + 1 < NST:
                          load_a(s + 1)
                      if tap == "a" and l == cfg.get("tap_layer", 0) and s == 0:
                          for kc in range(KC):
                              kb.op("dve", lambda e, kc=kc: e.tensor_copy(sq[:, kc, :], a_bf[:, kc, :]), reads=[a_bf.b], writes=[sq.b])
                          kb.dma("sp", dbg.rearrange("(kc p) t -> p kc t", p=128), sq[:], sq.b, reads=[sq.b], writes=[d_dbg])
                          stopf[0] = True
                          break
                      for qk in range(2):
                          dst, dbuf = (QT, d_QT) if qk == 0 else (KT, d_KT)
                          for ch in range(6):
                              c0 = qk * 768 + ch * 128
                              p1 = proj_fm(win, c0, a_bf)
                              p2 = proj_fm(wsw, c0, a_bf)
                              o = qo[(qk * 6 + ch) % 2]
                              kb.op("dve", lambda e, p1=p1, cst2=cst2: e.tensor_tensor(r1[:], p1[:], cst2[:, 0, :], ALU.mult),
                                    reads=[p1.b, cst2.b], writes=[r1.b])
                              kb.op("dve", lambda e, p2=p2, cst2=cst2: e.tensor_tensor(r2[:], p2[:], cst2[:, 1, :], ALU.mult),
                                    reads=[p2.b, cst2.b], writes=[r2.b])
                              kb.op("pool", lambda e, o=o: e.tensor_tensor(o[:], r1[:], r2[:], ALU.add),
                                    reads=[r1.b, r2.b], writes=[o.b])
                              kb.dma("sp", dst[ch * 128:(ch + 1) * 128, t0:t0 + ST], o[:], o.b, reads=[o.b], writes=[dbuf])
                      def do_mix(tk0, vgn):
                          pm = nps()
                          for g in range(4):
                              kb.op("pe", lambda e, g=g: e.matmul(pm[:, g * 128:(g + 1) * 128], vgn[:, g * 128:(g + 1) * 128],
                                                                   wst[:, g * 128:(g + 1) * 128], start=True, stop=False),
                                    reads=[vgn.b, wst.b], writes=[pm.b])
                              kb.op("pe", lambda e, g=g: e.matmul(pm[:, g * 128:(g + 1) * 128], ones_f[0:1, :],
                                                                   bsr[:, g * 128:(g + 1) * 128], start=False, stop=True),
                                    reads=[ones_f.b, bsr.b], writes=[pm.b])
                          kb.op("act", lambda e: e.copy(sq[:, 0:4, tk0:tk0 + 128], pm[:].rearrange("p (g t) -> p g t", g=4)),
                                reads=[pm.b], writes=[sq.b])

                      prev_mix = None
                      for tt in range(4):
                          tk0 = tt * 128
                          v_o = vo[tt % 2]
                          vgn = vgn2[tt % 2]
                          for (c0, n) in ((1536, 512), (2048, 256)):
                              p = nps()
                              for kc in range(KC):
                                  kb.op("pe", lambda e, kc=kc, p=p, c0=c0, n=n, tk0=tk0: e.matmul(
                                      p[:, 0:n], a_bf[:, kc, tk0:tk0 + 128], win[:, kc, c0:c0 + n],
                                      start=(kc == 0), stop=(kc == KC - 1)),
                                      reads=[a_bf.b, win.b], writes=[p.b])
                              kb.op("act", lambda e, p=p, c0=c0, n=n, v_o=v_o: e.copy(v_o[:, c0 - 1536:c0 - 1536 + n], p[:, 0:n]),
                                    reads=[p.b], writes=[v_o.b])
                          kb.dma("sp", Vd[t0 + tk0:t0 + tk0 + 128, :], v_o[:], v_o.b, reads=[v_o.b], writes=[d_V])
                          p = nps()
                          for kc in range(KC):
                              kb.op("pe", lambda e, kc=kc, p=p, tk0=tk0: e.matmul(
                                  p[:], a_bf[:, kc, tk0:tk0 + 128], win[:, kc, 2816:3328],
                                  start=(kc == 0), stop=(kc == KC - 1)),
                                  reads=[a_bf.b, win.b], writes=[p.b])
                          gelu_from(p[:], p.b, g3t[:], g3t.b, (g1t[:], g2t[:]))
                          kb.op("dve", lambda e: e.reduce_sum(st1[:, 0:1], g3t[:], AX.X), reads=[g3t.b], writes=[st1.b])
                          kb.op("dve", lambda e: e.tensor_scalar(st1[:, 1:2], st1[:, 0:1], -1.0 / 512, None, ALU.mult),
                                reads=[st1.b], writes=[st1.b])
                          kb.op("act", lambda e: e.activation(g1t[:], g3t[:], AF.Identity, bias=st1[:, 1:2]),
                                reads=[g3t.b, st1.b], writes=[g1t.b])
                          kb.op("act", lambda e: e.activation(g2t[:], g1t[:], AF.Square, accum_out=st1[:, 2:3]),
                                reads=[g1t.b], writes=[g2t.b, st1.b])
                          kb.op("act", lambda e: e.activation(st1[:, 3:4], st1[:, 2:3], AF.Sqrt, bias=eps_t[:, 1:2], scale=1.0 / 512),
                                reads=[st1.b, eps_t.b], writes=[st1.b])
                          kb.op("dve", lambda e: e.reciprocal(st1[:, 3:4], st1[:, 3:4]), reads=[st1.b], writes=[st1.b])
                          kb.op("dve", lambda e: e.scalar_tensor_tensor(g2t[:], g1t[:], st1[:, 3:4], lng[:], ALU.mult, ALU.mult),
                                reads=[g1t.b, st1.b, lng.b], writes=[g2t.b])
                          kb.op("dve", lambda e, vgn=vgn: e.tensor_tensor(vgn[:], g2t[:], lnb[:], ALU.add),
                                reads=[g2t.b, lnb.b], writes=[vgn.b])
                          if prev_mix is not None:
                              do_mix(*prev_mix)
                          prev_mix = (tk0, vgn)
                      do_mix(*prev_mix)
                      for g in range(4):
                          p = proj_fm(win, 2304 + g * 128, a_bf)
                          gelu_from(p[:], p.b, g3t[:], g3t.b, (g1t[:], g2t[:]))
                          kb.op("dve", lambda e, g=g: e.tensor_tensor(gmT[:, g, :], g3t[:], sq[:, g, :], ALU.mult),
                                reads=[g3t.b, sq.b], writes=[gmT.b])
                      if tap == "gm" and l == cfg.get("tap_layer", 0) and s == 0:
                          for g in range(4):
                              kb.op("dve", lambda e, g=g: e.tensor_copy(sq[:, 4 + g, :], gmT[:, g, :]), reads=[gmT.b], writes=[sq.b])
                          kb.dma("sp", dbg.rearrange("(kc p) t -> p kc t", p=128), sq[:, 4:8, :], sq.b, reads=[sq.b], writes=[d_dbg])
                          stopf[0] = True
                          break
                      for dc in range(KC):
                          p = proj_fm(win, 3328 + dc * 128, a_bf)
                          o = so[0]
                          kb.op("act", lambda e, p=p, o=o: e.activation(o[:], p[:], AF.Sigmoid), reads=[p.b], writes=[o.b])
                          kb.dma("sp", sigA[dc * 128:(dc + 1) * 128, t0:t0 + ST], o[:], o.b, reads=[o.b], writes=[d_sigA[s]])
                          p = proj_fm(win, 4352 + dc * 128, a_bf)
                          kb.op("act", lambda e, p=p: e.activation(r1[:], p[:], AF.Sigmoid), reads=[p.b], writes=[r1.b])
                          p2 = nps()
                          for kc in range(4):
                              kb.op("pe", lambda e, kc=kc, p2=p2, dc=dc: e.matmul(
                                  p2[:], wpg[:, kc, dc * 128:(dc + 1) * 128], gmT[:, kc, :], start=(kc == 0), stop=(kc == 3)),
                                  reads=[wpg.b, gmT.b], writes=[p2.b])
                          o2 = po[0]
                          kb.op("dve", lambda e, p2=p2, o2=o2: e.tensor_tensor(o2[:], p2[:], r1[:], ALU.mult),
                                reads=[p2.b, r1.b], writes=[o2.b])
                          kb.dma("sp", part2[dc * 128:(dc + 1) * 128, t0:t0 + ST], o2[:], o2.b, reads=[o2.b], writes=[d_part2[s]])
                  kb.barrier()

              if stopf[0]:
                  return
              if tap == "pa" and l == cfg.get("tap_layer", 0):
                  with ExitStack() as ph:
                      tt = sb(ph, "tapt", [128, KC, ST])
                      for s in range(NST):
                          kb.dma("sp", tt[:], part2[:, s * ST:(s + 1) * ST].rearrange("(kc p) t -> p kc t", p=128), tt.b,
                                 reads=[d_part2[s], d_dbg], writes=[tt.b])
                          kb.dma("sp", dbg[:, s * ST:(s + 1) * ST].rearrange("(kc p) t -> p kc t", p=128), tt[:], tt.b,
                                 reads=[tt.b], writes=[d_dbg])
                      kb.barrier()
                  return
              with ExitStack() as ph:
                  numT = sb(ph, "numT", [128, 2, S])
                  denT = sb(ph, "denT", [128, 2, S])
                  pha = ExitStack()
                  qc = [sb(pha, "qc%d" % i, [128, S], BF16) for i in range(2)]
                  kz = [[sb(pha, "kz%d_%d" % (i, hh), [128, S], BF16) for hh in range(2)] for i in range(2)]
                  vz = [sb(pha, "vz%d" % i, [128, 2, 128], BF16) for i in range(5)]
                  onz = sb(pha, "onz", [128, 2, 128], BF16)
                  am = sb(pha, "am", [128, 512], BF16)
                  amf = sb(pha, "amf", [128, 512])
                  pT = [sb(pha, "pT%d" % i, [128, 256], BF16) for i in range(6)]
                  for v in vz:
                      kb.op("pool", lambda e, v=v: e.memset(v[:], 0.0), writes=[v.b])
                  for i in range(2):
                      for hh in range(2):
                          kb.op("pool", lambda e, i=i, hh=hh: e.memset(kz[i][hh][:], 0.0), writes=[kz[i][hh].b])
                  kb.op("pool", lambda e: e.memset(onz[:], 0.0), writes=[onz.b])
                  kb.op("pool", lambda e: e.memset(onz[:, 0, 0:64], 1.0), writes=[onz.b])
                  kb.op("pool", lambda e: e.memset(onz[:, 1, 64:128], 1.0), writes=[onz.b])
                  kb.dma("sp", amf[:], attm, amf.b, writes=[amf.b])
                  kb.op("dve", lambda e: e.tensor_copy(am[:], amf[:]), reads=[amf.b], writes=[am.b])
                  kb.barrier()

                  att_groups = cfg.get("att_groups", [0, 1, 2])
                  cnts = {"it": 0, "v": 0, "p": 0}

                  def stage1(g, r, c, q_t, k_t, f0, j, n, vprev):
                      tok0 = n * 128 * r + j
                      v_t = vz[cnts["v"] % 5]
                      cnts["v"] += 1
                      vsrc = Vd[tok0:tok0 + 127 * r + 1:r, f0:f0 + 128].rearrange("k (h d) -> k h d", h=2)
                      for hh in range(2):
                          kb.dma("sp", v_t[:, hh, hh * 64:(hh + 1) * 64], vsrc[:, hh, :], v_t.b, reads=[d_V], writes=[v_t.b])
                      qsl = slice(tok0, tok0 + 127 * r + 1, r)
                      kbs = []
                      if n > 0:
                          kbs.append((slice(tok0 - 128 * r, tok0 - r + 1, r), vprev, 0))
                      kbs.append((qsl, v_t, 1))
                      pts = []
                      for (ksl, vt_, mi) in kbs:
                          pss = PS[cnts["p"] % 4]
                          p_t = pT[cnts["p"] % 6]
                          cnts["p"] += 1
                          for hh in range(2):
                              kb.op("pe", lambda e, hh=hh, pss=pss, ksl=ksl: e.matmul(
                                  pss[:, hh * 128:(hh + 1) * 128], k_t[hh][:, ksl], q_t[:, qsl], start=True, stop=True),
                                  reads=[k_t[hh].b, q_t.b], writes=[pss.b])
                          kb.op("act", lambda e, pss=pss, p_t=p_t: e.activation(p_t[:], pss[:, 0:256], AF.Exp, scale=0.125),
                                reads=[pss.b], writes=[p_t.b])
                          kb.op("pool", lambda e, p_t=p_t, mi=mi: e.tensor_tensor(p_t[:], p_t[:], am[:, mi * 256:(mi + 1) * 256], ALU.mult),
                                reads=[p_t.b, am.b], writes=[p_t.b])
                          pts.append((p_t, vt_))
                      return (g, c, qsl, pts), v_t

                  def stage2(unit):
                      g, c, qsl, pts = unit
                      po_ = PS[4 + (cnts["it"] % 2)]
                      pd_ = PS[6 + (cnts["it"] % 2)]
                      cnts["it"] += 1
                      for bi, (p_t, vt_) in enumerate(pts):
                          first = (bi == 0)
                          last = (bi == len(pts) - 1)
                          for hh in range(2):
                              kb.op("pe", lambda e, hh=hh, vt_=vt_, p_t=p_t, first=first, last=last: e.matmul(
                                  po_[:, 0:128], vt_[:, hh, :], p_t[:, hh * 128:(hh + 1) * 128],
                                  start=(first and hh == 0), stop=(last and hh == 1)),
                                  reads=[vt_.b, p_t.b], writes=[po_.b])
                          for hh in range(2):
                              kb.op("pe", lambda e, hh=hh, p_t=p_t, first=first, last=last: e.matmul(
                                  pd_[:, 0:128], onz[:, hh, :], p_t[:, hh * 128:(hh + 1) * 128],
                                  start=(first and hh == 0), stop=(last and hh == 1)),
                                  reads=[onz.b, p_t.b], writes=[pd_.b])
                      if g == att_groups[0]:
                          kb.op("dve", lambda e: e.tensor_copy(numT[:, c, qsl], po_[:, 0:128]), reads=[po_.b], writes=[numT.b])
                          kb.op("dve", lambda e: e.tensor_copy(denT[:, c, qsl], pd_[:, 0:128]), reads=[pd_.b], writes=[denT.b])
                      else:
                          kb.op("dve", lambda e: e.tensor_tensor(numT[:, c, qsl], po_[:, 0:128], numT[:, c, qsl], ALU.add),
                                reads=[po_.b, numT.b], writes=[numT.b])
                          kb.op("dve", lambda e: e.tensor_tensor(denT[:, c, qsl], pd_[:, 0:128], denT[:, c, qsl], ALU.add),
                                reads=[pd_.b, denT.b], writes=[denT.b])

                  pending = None
                  for g, (win_, r) in enumerate(GROUPS):
                      if g not in att_groups:
                          continue
                      L = S // r
                      nb = L // 128
                      for c in range(2):
                          q_t = qc[(g * 2 + c) % 2]
                          k_t = kz[(g * 2 + c) % 2]
                          f0 = g * 256 + c * 128
                          kb.dma("sp", q_t[:], QT[f0:f0 + 128, :], q_t.b, reads=[d_QT], writes=[q_t.b])
                          for hh in range(2):
                              kb.dma("sp", k_t[hh][hh * 64:(hh + 1) * 64, :], KT[f0 + hh * 64:f0 + (hh + 1) * 64, :], k_t[hh].b,
                                     reads=[d_KT], writes=[k_t[hh].b])
                          for j in range(r):
                              vprev = None
                              for n in range(min(nb, cfg.get("att_nblk", 10 ** 6))):
                                  unit, vprev = stage1(g, r, c, q_t, k_t, f0, j, n, vprev)
                                  if pending is not None:
                                      stage2(pending)
                                  pending = unit
                  if pending is not None:
                      stage2(pending)
                  kb.barrier()
                  pha.close()

                  if tap == "attn" and l == cfg.get("tap_layer", 0):
                      kb.dma("sp", dbg[0:256, :].rearrange("(c p) t -> p c t", p=128), numT[:], numT.b, reads=[numT.b], writes=[d_dbg])
                      kb.dma("sp", dbg[256:512, :].rearrange("(c p) t -> p c t", p=128), denT[:], denT.b, reads=[denT.b], writes=[d_dbg])
                      kb.barrier()
                      stopf[0] = True

                  with ExitStack() as ph2:
                      wpa = sb(ph2, "wpa", [128, 2, D], BF16)
                      wo = sb(ph2, "wo", [128, KC, D], BF16)
                      attn = sb(ph2, "attn", [128, 2, ST], BF16)
                      sgt = [sb(ph2, "sgt%d" % i, [128, KC, ST]) for i in range(2)]
                      p2t = [sb(ph2, "p2t%d" % i, [128, KC, ST]) for i in range(2)]
                      hh_t = [sb(ph2, "hh_t%d" % i, [128, KC, ST]) for i in range(2)]
                      mg = sb(ph2, "mg", [128, KC, ST], BF16)
                      tm = sb(ph2, "tm", [128, ST])
                      wpav = w_pa[l * 256:(l + 1) * 256, :].rearrange("(kc p) n -> p kc n", p=128)
                      for kc in range(2):
                          kb.dma("pool", wpa[:, kc, :], wpav[:, kc, :], wpa.b, writes=[wpa.b])
                      wov = w_o[l * D:(l + 1) * D, :].rearrange("(kc p) n -> p kc n", p=128)
                      for kc in range(KC):
                          kb.dma("pool", wo[:, kc, :], wov[:, kc, :], wo.b, writes=[wo.b])
                      def load_b2(s_):
                          t0_ = s_ * ST
                          kb.dma("sp", sgt[s_ % 2][:], sigA[:, t0_:t0_ + ST].rearrange("(kc p) t -> p kc t", p=128), sgt[s_ % 2].b,
                                 reads=[d_sigA[s_]], writes=[sgt[s_ % 2].b])
                          kb.dma("sp", p2t[s_ % 2][:], part2[:, t0_:t0_ + ST].rearrange("(kc p) t -> p kc t", p=128), p2t[s_ % 2].b,
                                 reads=[d_part2[s_]], writes=[p2t[s_ % 2].b])
                          kb.dma("sp", hh_t[s_ % 2][:], src_h[:, t0_:t0_ + ST].rearrange("(kc p) t -> p kc t", p=128), hh_t[s_ % 2].b,
                                 reads=[d_hT[s_]], writes=[hh_t[s_ % 2].b])

                      for s in (range(NST) if not stopf[0] else []):
                          t0 = s * ST
                          sg = sgt[s % 2]
                          p2 = p2t[s % 2]
                          hh2 = hh_t[s % 2]
                          if s == 0:
                              load_b2(0)
                          if s + 1 < NST:
                              load_b2(s + 1)
                          for c in range(2):
                              kb.op("dve", lambda e, c=c, t0=t0: e.reciprocal(tm[:], denT[:, c, t0:t0 + ST]), reads=[denT.b], writes=[tm.b])
                              kb.op("dve", lambda e, c=c, t0=t0: e.tensor_tensor(attn[:, c, :], numT[:, c, t0:t0 + ST], tm[:], ALU.mult),
                                    reads=[numT.b, tm.b], writes=[attn.b])
                          for dc in range(KC):
                              p = PS[dc % 6]
                              for c in range(2):
                                  kb.op("pe", lambda e, c=c, p=p, dc=dc: e.matmul(p[:], wpa[:, c, dc * 128:(dc + 1) * 128], attn[:, c, :],
                                                                                   start=(c == 0), stop=(c == 1)),
                                        reads=[wpa.b, attn.b], writes=[p.b])
                              kb.op("dve", lambda e, p=p, dc=dc, sg=sg: e.tensor_tensor(tm[:], p[:], sg[:, dc, :], ALU.mult),
                                    reads=[p.b, sg.b], writes=[tm.b])
                              kb.op("pool", lambda e, dc=dc, p2=p2: e.tensor_tensor(mg[:, dc, :], tm[:], p2[:, dc, :], ALU.add),
                                    reads=[tm.b, p2.b], writes=[mg.b])
                          for dc in range(KC):
                              p = PS[dc % 6]
                              for kc in range(KC):
                                  kb.op("pe", lambda e, kc=kc, p=p, dc=dc: e.matmul(p[:], wo[:, kc, dc * 128:(dc + 1) * 128], mg[:, kc, :],
                                                                                     start=(kc == 0), stop=(kc == KC - 1)),
                                        reads=[wo.b, mg.b], writes=[p.b])
                              kb.op("dve", lambda e, p=p, dc=dc, hh2=hh2: e.scalar_tensor_tensor(hh2[:, dc, :], p[:], modv[:, 16 + dc:17 + dc],
                                                                                         hh2[:, dc, :], ALU.mult, ALU.add),
                                    reads=[p.b, modv.b, hh2.b], writes=[hh2.b])
                          kb.dma("sp", hT[:, t0:t0 + ST].rearrange("(kc p) t -> p kc t", p=128), hh2[:], hh2.b,
                                 reads=[hh2.b], writes=[d_hT[s]])
                      kb.barrier()

              if stopf[0]:
                  return
              if tap == "h1" and l == cfg.get("tap_layer", 0):
                  with ExitStack() as ph:
                      tt = sb(ph, "tapt", [128, KC, ST])
                      for s in range(NST):
                          kb.dma("sp", tt[:], hT[:, s * ST:(s + 1) * ST].rearrange("(kc p) t -> p kc t", p=128), tt.b,
                                 reads=[d_hT[s], d_dbg], writes=[tt.b])
                          kb.dma("sp", dbg[:, s * ST:(s + 1) * ST].rearrange("(kc p) t -> p kc t", p=128), tt[:], tt.b,
                                 reads=[tt.b], writes=[d_dbg])
                      kb.barrier()
                  return

              if do_moe:
                  with ExitStack() as ph:
                      SL = 1024
                      hb = sb(ph, "hb", [128, KC, ST])
                      sqc = sb(ph, "sq2", [128, KC, ST])
                      rstdc = sb(ph, "rstd2", [128, ST])
                      fT = sb(ph, "fT", [128, KC, SL], BF16)
                      yacc = sb(ph, "yacc", [128, KC, SL])
                      w1p = [sb(ph, "w1p%d" % i, [128, KC, 512], BF16) for i in range(3)]
                      w2e = [sb(ph, "w2e%d" % i, [128, KC, D], BF16) for i in range(2)]
                      actT = [sb(ph, "actT%d" % i, [128, KC, ST], BF16) for i in range(2)]
                      b1t = sb(ph, "b1t", [128, E * 16])
                      b2t = sb(ph, "b2t", [32, D])
                      wr = sb(ph, "wr", [128, KC, E])
                      brt = sb(ph, "brt", [128, E])
                      GT = sb(ph, "GT", [32, SL])
                      sel = sb(ph, "sel", [32, E * 128], BF16)
                      GTh = sb(ph, "GTh", [32, SL], BF16)
                      GTl = sb(ph, "GTl", [32, SL], BF16)
                      GTf = sb(ph, "GTf", [32, SL])
                      lg = sb(ph, "lg", [128, E])
                      mx8 = sb(ph, "mx8", [128, 8])
                      ex = sb(ph, "ex", [128, E])
                      mk = sb(ph, "mk", [128, E])
                      sm = sb(ph, "sm", [128, 2])
                      xg = [sb(ph, "xg%d" % i, [128, ST]) for i in range(2)]
                      sgm = [sb(ph, "sgm%d" % i, [128, ST]) for i in range(2)]
                      xl = [sb(ph, "xl%d" % i, [128, ST]) for i in range(2)]

                      kb.dma("sp", b1t[:], b1T[l * 128:(l + 1) * 128, :], b1t.b, writes=[b1t.b])
                      kb.dma("sp", b2t[:], b2[l * E:(l + 1) * E, :], b2t.b, writes=[b2t.b])
                      kb.dma("sp", wr[:], w_router[l * D:(l + 1) * D, :].rearrange("(kc p) n -> p kc n", p=128), wr.b, writes=[wr.b])
                      kb.dma("sp", brt[:], b_router[l:l + 1, :].partition_broadcast(128), brt.b, writes=[brt.b])
                      kb.op("pool", lambda e: e.iota(sel[:], [[1, E], [0, 128]], base=0, channel_multiplier=-1,
                                                     allow_small_or_imprecise_dtypes=True), writes=[sel.b])
                      kb.op("dve", lambda e: e.tensor_scalar(sel[:], sel[:], 0.0, None, ALU.is_equal), reads=[sel.b], writes=[sel.b])

                      gbs = [sb(ph, "gbs%d" % i, [128, ST]) for i in range(2)]
                      nsl = S // SL

                      def issue_w1(k):
                          if k >= nsl * n_exp * 4:
                              return
                          ex_i = (k // 4) % n_exp
                          q = k % 4
                          wp = w1p[k % 3]
                          w1v = w1[(l * E + ex_i) * D:(l * E + ex_i + 1) * D, :].rearrange("(kc p) n -> p kc n", p=128)
                          kb.dma("pool", wp[:], w1v[:, :, q * 512:(q + 1) * 512], wp.b, writes=[wp.b])

                      def issue_w2(m):
                          if m >= nsl * n_exp:
                              return
                          ex_i = m % n_exp
                          w2t = w2e[m % 2]
                          w2v = w2[(l * E + ex_i) * D:(l * E + ex_i + 1) * D, :].rearrange("(kc p) n -> p kc n", p=128)
                          kb.dma("pool", w2t[:], w2v, w2t.b, writes=[w2t.b])

                      issue_w1(0)
                      issue_w1(1)
                      issue_w2(0)
                      for sl in range(S // SL):
                          for half in range(2):
                              t0 = sl * SL + half * ST
                              s = t0 // ST
                              kb.dma("sp", hb[:], hT[:, t0:t0 + ST].rearrange("(kc p) t -> p kc t", p=128), hb.b,
                                     reads=[d_hT[s]], writes=[hb.b])
                              rmsnorm_to((sqc, rstdc), hb, 8, 24, fT, a_f32=None, a_off=half * ST)
                              for kc in range(KC):
                                  kb.op("act", lambda e, kc=kc: e.activation(sqc[:, kc, :], sqc[:, kc, :], AF.Identity,
                                                                              bias=modv[:, 24 + kc:25 + kc], scale=wv[:, 8 + kc:9 + kc]),
                                        reads=[sqc.b, modv.b, wv.b], writes=[sqc.b])
                              for tt in range(4):
                                  pl = PS[tt % 2]
                                  for kc in range(KC):
                                      kb.op("pe", lambda e, kc=kc, pl=pl, tt=tt: e.matmul(pl[:, 0:E], sqc[:, kc, tt * 128:(tt + 1) * 128], wr[:, kc, :],
                                                                                           start=(kc == 0), stop=(kc == KC - 1)),
                                            reads=[sqc.b, wr.b], writes=[pl.b])
                                  kb.op("dve", lambda e, pl=pl: e.tensor_tensor(lg[:], pl[:, 0:E], brt[:], ALU.add),
                                        reads=[pl.b, brt.b], writes=[lg.b])
                                  kb.op("dve", lambda e: e.max(mx8[:], lg[:]), reads=[lg.b], writes=[mx8.b])
                                  kb.op("dve", lambda e: e.tensor_scalar(mk[:], lg[:], mx8[:, 3:4], None, ALU.is_ge),
                                        reads=[lg.b, mx8.b], writes=[mk.b])
                                  kb.op("dve", lambda e: e.tensor_scalar(sm[:, 0:1], mx8[:, 0:1], -1.0, None, ALU.mult),
                                        reads=[mx8.b], writes=[sm.b])
                                  kb.op("act", lambda e: e.activation(ex[:], lg[:], AF.Exp, bias=sm[:, 0:1]),
                                        reads=[lg.b, sm.b], writes=[ex.b])
                                  kb.op("dve", lambda e: e.tensor_tensor(ex[:], ex[:], mk[:], ALU.mult), reads=[ex.b, mk.b], writes=[ex.b])
                                  kb.op("dve", lambda e: e.reduce_sum(sm[:, 1:2], ex[:], AX.X), reads=[ex.b], writes=[sm.b])
                                  kb.op("dve", lambda e: e.reciprocal(sm[:, 1:2], sm[:, 1:2]), reads=[sm.b], writes=[sm.b])
                                  kb.op("dve", lambda e: e.tensor_scalar(ex[:], ex[:], sm[:, 1:2], None, ALU.mult),
                                        reads=[ex.b, sm.b], writes=[ex.b])
                                  pt = PS[2 + tt % 2]
                                  kb.op("pe", lambda e, pt=pt: e.transpose(pt[0:32, 0:128], ex[:], ident[:]),
                                        reads=[ex.b, ident.b], writes=[pt.b])
                                  c0 = half * ST + tt * 128
                                  kb.op("act", lambda e, pt=pt, c0=c0: e.copy(GT[:, c0:c0 + 128], pt[0:32, 0:128]),
                                        reads=[pt.b], writes=[GT.b])
                          kb.op("dve", lambda e: e.tensor_copy(GTh[:], GT[:]), reads=[GT.b], writes=[GTh.b])
                          kb.op("dve", lambda e: e.tensor_copy(GTf[:], GTh[:]), reads=[GTh.b], writes=[GTf.b])
                          kb.op("dve", lambda e: e.tensor_tensor(GTf[:], GT[:], GTf[:], ALU.subtract), reads=[GT.b, GTf.b], writes=[GTf.b])
                          kb.op("dve", lambda e: e.tensor_copy(GTl[:], GTf[:]), reads=[GTf.b], writes=[GTl.b])
                          for half in range(2):
                              for dc in range(KC):
                                  p = PS[4 + dc % 2]
                                  kb.op("pe", lambda e, p=p, dc=dc, half=half: e.matmul(p[:], b2t[:, dc * 128:(dc + 1) * 128],
                                                                                         GT[:, half * ST:(half + 1) * ST], start=True, stop=True),
                                        reads=[b2t.b, GT.b], writes=[p.b])
                                  kb.op("act", lambda e, p=p, dc=dc, half=half: e.copy(yacc[:, dc, half * ST:(half + 1) * ST], p[:]),
                                        reads=[p.b], writes=[yacc.b])
                          for ex_i in range(n_exp):
                              m_idx = sl * n_exp + ex_i
                              w2t = w2e[m_idx % 2]
                              issue_w2(m_idx + 1)
                              for q in range(4):
                                  k_idx = m_idx * 4 + q
                                  wp = w1p[k_idx % 3]
                                  issue_w1(k_idx + 2)
                                  for half in range(2):
                                      hs = slice(half * ST, (half + 1) * ST)
                                      if q == 0:
                                          pg_ = PS[6 + half]
                                          kb.op("pe", lambda e, pg_=pg_, ex_i=ex_i, hs=hs: e.matmul(
                                              pg_[:], sel[:, ex_i * 128:(ex_i + 1) * 128], GTh[:, hs], start=True, stop=False),
                                              reads=[sel.b, GTh.b], writes=[pg_.b])
                                          kb.op("pe", lambda e, pg_=pg_, ex_i=ex_i, hs=hs: e.matmul(
                                              pg_[:], sel[:, ex_i * 128:(ex_i + 1) * 128], GTl[:, hs], start=False, stop=True),
                                              reads=[sel.b, GTl.b], writes=[pg_.b])
                                          kb.op("act", lambda e, pg_=pg_, half=half: e.copy(gbs[half][:], pg_[:]),
                                                reads=[pg_.b], writes=[gbs[half].b])
                                      for jl in range(2):
                                          jc = q * 2 + jl
                                          pgl = PS[(jl * 2) % 4]
                                          pll = PS[(jl * 2 + 1) % 4]
                                          for kc in range(KC):
                                              kb.op("pe", lambda e, kc=kc, pgl=pgl, wp=wp, jl=jl, hs=hs: e.matmul(
                                                  pgl[:], wp[:, kc, jl * 256:(jl + 1) * 256:2], fT[:, kc, hs],
                                                  start=(kc == 0), stop=(kc == KC - 1)),
                                                  reads=[wp.b, fT.b], writes=[pgl.b])
                                          for kc in range(KC):
                                              kb.op("pe", lambda e, kc=kc, pll=pll, wp=wp, jl=jl, hs=hs: e.matmul(
                                                  pll[:], wp[:, kc, jl * 256 + 1:(jl + 1) * 256:2], fT[:, kc, hs],
                                                  start=(kc == 0), stop=(kc == KC - 1)),
                                                  reads=[wp.b, fT.b], writes=[pll.b])
                                          bcol = ex_i * 16 + jc * 2
                                          x_g = xg[jl]
                                          s_g = sgm[jl]
                                          x_l = xl[jl]
                                          a_t = actT[half]
                                          g_b = gbs[half]
                                          kb.op("act", lambda e, pgl=pgl, x_g=x_g, bcol=bcol: e.activation(
                                              x_g[:], pgl[:], AF.Identity, bias=b1t[:, bcol:bcol + 1]),
                                              reads=[pgl.b, b1t.b], writes=[x_g.b])
                                          kb.op("act", lambda e, pll=pll, x_l=x_l, bcol=bcol: e.activation(
                                              x_l[:], pll[:], AF.Identity, bias=b1t[:, bcol + 1:bcol + 2]),
                                              reads=[pll.b, b1t.b], writes=[x_l.b])
                                          kb.op("pool", lambda e, x_g=x_g: e.tensor_scalar(x_g[:], x_g[:], 7.0, -1.0e30, ALU.min, ALU.max),
                                                reads=[x_g.b], writes=[x_g.b])
                                          kb.op("act", lambda e, x_g=x_g, s_g=s_g: e.activation(s_g[:], x_g[:], AF.Sigmoid, scale=1.702),
                                                reads=[x_g.b], writes=[s_g.b])
                                          kb.op("pool", lambda e, x_l=x_l: e.tensor_scalar(x_l[:], x_l[:], 7.0, -7.0, ALU.min, ALU.max),
                                                reads=[x_l.b], writes=[x_l.b])
                                          kb.op("pool", lambda e, s_g=s_g, g_b=g_b: e.tensor_tensor(s_g[:], s_g[:], g_b[:], ALU.mult),
                                                reads=[s_g.b, g_b.b], writes=[s_g.b])
                                          kb.op("dve", lambda e, x_g=x_g, s_g=s_g: e.tensor_tensor(x_g[:], x_g[:], s_g[:], ALU.mult),
                                                reads=[x_g.b, s_g.b], writes=[x_g.b])
                                          kb.op("dve", lambda e, x_g=x_g, x_l=x_l, a_t=a_t, jc=jc: e.scalar_tensor_tensor(
                                              a_t[:, jc, :], x_l[:], 1.0, x_g[:], ALU.add, ALU.mult),
                                              reads=[x_g.b, x_l.b], writes=[a_t.b])
                              for half in range(2):
                                  hs = slice(half * ST, (half + 1) * ST)
                                  a_t = actT[half]
                                  for dc in range(KC):
                                      p = PS[4 + dc % 2]
                                      for jc in range(KC):
                                          kb.op("pe", lambda e, jc=jc, p=p, dc=dc, a_t=a_t, w2t=w2t: e.matmul(
                                              p[:], w2t[:, jc, dc * 128:(dc + 1) * 128], a_t[:, jc, :],
                                              start=(jc == 0), stop=(jc == KC - 1)),
                                              reads=[w2t.b, a_t.b], writes=[p.b])
                                      kb.op("dve", lambda e, p=p, dc=dc, hs=hs: e.tensor_tensor(yacc[:, dc, hs], p[:], yacc[:, dc, hs], ALU.add),
                                            reads=[p.b, yacc.b], writes=[yacc.b])
                          for half in range(2):
                              t0 = sl * SL + half * ST
                              s = t0 // ST
                              kb.dma("sp", hb[:], hT[:, t0:t0 + ST].rearrange("(kc p) t -> p kc t", p=128), hb.b,
                                     reads=[d_hT[s]], writes=[hb.b])
                              for dc in range(KC):
                                  kb.op("dve", lambda e, dc=dc, half=half: e.scalar_tensor_tensor(
                                      hb[:, dc, :], yacc[:, dc, half * ST:(half + 1) * ST], modv[:, 40 + dc:41 + dc], hb[:, dc, :], ALU.mult, ALU.add),
                                      reads=[yacc.b, modv.b, hb.b], writes=[hb.b])
                              kb.dma("sp", hT[:, t0:t0 + ST].rearrange("(kc p) t -> p kc t", p=128), hb[:], hb.b,
                                     reads=[hb.b], writes=[d_hT[s]])
                      kb.barrier()

        for l in range(n_layers):
            one_layer(l)
            if stopf[0] or (tap is not None and l == cfg.get("tap_layer", 0)):
                break

        if tap is None:
            with ExitStack() as ph:
                hb = [sb(ph, "fhb%d" % i, [128, KC, ST]) for i in range(2)]
                sq = sb(ph, "fsq", [128, KC, ST])
                rstd = sb(ph, "frstd", [128, ST])
                fgt = sb(ph, "fgt", [128, KC])
                kb.dma("sp", fgt[:], fg, fgt.b, writes=[fgt.b])
                for s in range(NST):
                    t0 = s * ST
                    h_ = hb[s % 2]
                    ps = PS[s % 2]
                    kb.dma("sp", h_[:], (hT if n_layers > 0 else xT)[:, t0:t0 + ST].rearrange("(kc p) t -> p kc t", p=128), h_.b,
                           reads=[d_hT[s]], writes=[h_.b])
                    kb.op("act", lambda e, h_=h_: e.activation(sq[:], h_[:], AF.Square), reads=[h_.b], writes=[sq.b])
                    for kc in range(KC):
                        kb.op("pe", lambda e, kc=kc, ps=ps: e.matmul(ps[:], ones_f[:], sq[:, kc, :], start=(kc == 0), stop=(kc == KC - 1)),
                              reads=[ones_f.b, sq.b], writes=[ps.b])
                    kb.op("act", lambda e, ps=ps: e.activation(rstd[:], ps[:], AF.Sqrt, bias=eps_t[:, 0:1], scale=1.0 / D),
                          reads=[ps.b, eps_t.b], writes=[rstd.b])
                    kb.op("dve", lambda e: e.reciprocal(rstd[:], rstd[:]), reads=[rstd.b], writes=[rstd.b])
                    for kc in range(KC):
                        kb.op("dve", lambda e, kc=kc, h_=h_: e.scalar_tensor_tensor(h_[:, kc, :], h_[:, kc, :], fgt[:, kc:kc + 1], rstd[:],
                                                                                     ALU.mult, ALU.mult),
                              reads=[h_.b, fgt.b, rstd.b], writes=[h_.b])
                    kb.dma("sp", outT[:, t0:t0 + ST].rearrange("(kc p) t -> p kc t", p=128), h_[:], h_.b,
                           reads=[h_.b], writes=[d_out])
                kb.barrier()
        else:
            with ExitStack() as ph:
                z = sb(ph, "ztile", [128, 512])
                kb.dma("sp", z[:], xT[0:128, 0:512], z.b, writes=[z.b])
                kb.dma("sp", outT[0:128, 0:512], z[:], z.b, reads=[z.b], writes=[d_out])
                kb.barrier()

        kb.emit()
    return nc


def _swap_perm():
    idx = np.arange(1536)
    d = idx % 64
    return (idx - d) + (d + 32) % 64


def make_in_maps(inputs, ncores=NCORES):
    f = lambda a: np.ascontiguousarray(np.asarray(a, dtype=np.float32))
    x = np.asarray(inputs["x"], dtype=np.float32)
    c = np.asarray(inputs["c"], dtype=np.float32)
    pos = np.asarray(inputs["positions"]).astype(np.int32)
    w_in = f(inputs["w_in"])
    half = 32
    inv_freq = (np.float32(10000.0) ** (-np.arange(half, dtype=np.float32) / np.float32(half))).astype(np.float32)
    cst = np.zeros((128, 4), np.float32)
    p = np.arange(128)
    cst[:, 0] = inv_freq[p % 32]
    cst[:, 1] = np.where((p % 64) < 32, -1.0, 1.0)
    tri = (np.arange(128)[:, None] <= np.arange(128)[None, :]).astype(np.float32)
    maskT = np.tile(tri, (1, 4))
    kq = np.arange(128)
    prev = (kq[None, :] <= kq[:, None]).astype(np.float32)
    cur = (kq[:, None] <= kq[None, :]).astype(np.float32)
    attm = np.concatenate([prev, prev, cur, cur], axis=1)
    perm = _swap_perm()
    shared = {
        "cst": cst,
        "w_ada": f(inputs["w_ada"]).reshape(DEPTH * D, 6 * D),
        "b_adaT": f(np.asarray(inputs["b_ada"], np.float32).reshape(DEPTH, 48, 128).transpose(0, 2, 1)).reshape(DEPTH * 128, 48),
        "n1g": f(np.asarray(inputs["norm1_g"], np.float32).reshape(DEPTH, KC, 128).transpose(0, 2, 1)).reshape(DEPTH * 128, KC),
        "n2g": f(np.asarray(inputs["norm2_g"], np.float32).reshape(DEPTH, KC, 128).transpose(0, 2, 1)).reshape(DEPTH * 128, KC),
        "fg": f(np.asarray(inputs["final_g"], np.float32).reshape(KC, 128).T),
        "w_in": w_in.reshape(DEPTH * D, INW),
        "w_in_sw": f(w_in[:, :, :1536][:, :, perm]).reshape(DEPTH * D, 1536),
        "w_sT": f(np.asarray(inputs["w_s"], np.float32).transpose(0, 3, 1, 2)).reshape(DEPTH * 128, 512),
        "maskT": maskT,
        "attm": attm,
        "b_s": f(inputs["b_s"]).reshape(DEPTH, 512),
        "ln_g": f(inputs["ln_g"]),
        "ln_b": f(inputs["ln_b"]),
        "w_pa": f(inputs["w_pa"]).reshape(DEPTH * 256, D),
        "w_pg": f(inputs["w_pg"]).reshape(DEPTH * 512, D),
        "w_o": f(inputs["w_o"]).reshape(DEPTH * D, D),
        "w_router": f(inputs["w_router"]).reshape(DEPTH * D, E),
        "b_router": f(inputs["b_router"]),
        "w1": f(inputs["w1"]).reshape(DEPTH * E * D, 2 * D),
        "b1T": f(np.asarray(inputs["b1"], np.float32).reshape(DEPTH, E, 8, 128, 2).transpose(0, 3, 1, 2, 4)).reshape(DEPTH * 128, E * 16),
        "w2": f(inputs["w2"]).reshape(DEPTH * E * D, D),
        "b2": f(inputs["b2"]).reshape(DEPTH * E, D),
    }
    maps = []
    for b in range(ncores):
        m = dict(shared)
        m["xT"] = f(x[b].T)
        m["cT"] = f(c[b].reshape(KC, 128).T)
        m["pos"] = np.ascontiguousarray(pos[b].reshape(1, S))
        maps.append(m)
    return maps


def kernel(**inputs):
    nc = build_nc()
    maps = make_in_maps(inputs)
    res = run_bass_kernel_spmd(nc, maps, core_ids=list(range(NCORES)))
    out = np.stack([np.ascontiguousarray(r["outT"].T) for r in res.results], axis=0)
    return out.astype(np.float32)
```

```python
import math
from contextlib import ExitStack

import numpy as np
import concourse.bass as bass
import concourse.mybir as mybir
from concourse.bass_utils import run_bass_kernel_spmd

F32 = mybir.dt.float32
BF16 = mybir.dt.bfloat16
I32 = mybir.dt.int32
ALU = mybir.AluOpType
AF = mybir.ActivationFunctionType
AX = mybir.AxisListType

D = 1024
S = 4096
NCORES = 8
DEPTH = 2
KC = 8
ST = 512
NST = S // ST
E = 32
INW = 5376
RMS_EPS = 1e-5
LN_EPS = 1e-5
GROUPS = ((128, 1), (512, 4), (2048, 16))
TWO_PI = 2.0 * math.pi


class Buf:
    __slots__ = ("name", "w", "r")

    def __init__(self, name):
        self.name = name
        self.w = None
        self.r = []


class Op:
    __slots__ = ("eng", "fn", "deps", "needed", "cnt", "is_dma", "sem", "semval")


class KB:
    ENGS = ("pe", "act", "dve", "pool", "sp")

    def __init__(self, nc, stack):
        self.nc = nc
        self.stack = stack
        self.ops = {e: [] for e in self.ENGS}
        self.last_c = {e: None for e in self.ENGS}
        self.dmas = []
        self.dma_sems = {}
        self.free_sems = {"sw": [], "hw": []}
        self.esem = {}
        for e in self.ENGS:
            self.esem[e] = stack.enter_context(nc.semaphore("es_" + e))
        self.nsem = 5

    def _mk(self, eng, fn, reads, writes, is_dma):
        op = Op()
        op.eng = eng
        op.fn = fn
        op.needed = False
        op.cnt = 0
        op.is_dma = is_dma
        op.sem = None
        op.semval = 0
        deps = []
        for b in reads:
            if b.w is not None:
                deps.append(b.w)
        for b in writes:
            if b.w is not None:
                deps.append(b.w)
            deps.extend(b.r)
        seen = set()
        out = []
        for d in deps:
            if id(d) in seen:
                continue
            seen.add(id(d))
            if (not d.is_dma) and (not is_dma) and d.eng == "pe" and eng == "pe":
                continue
            out.append(d)
        op.deps = out
        for b in reads:
            if not is_dma:
                b.r = [x for x in b.r if x.is_dma or x.eng != eng]
            b.r.append(op)
        for b in writes:
            b.w = op
            b.r = []
        self.ops[eng].append(op)
        if is_dma:
            self.dmas.append(op)
        else:
            self.last_c[eng] = op
        return op

    def op(self, eng, fn, reads=(), writes=()):
        return self._mk(eng, fn, reads, writes, False)

    def dma(self, q, out, in_, sb, reads=(), writes=()):
        op = self._mk(q, lambda e: e.dma_start(out=out, in_=in_), reads, writes, True)
        qt = "sw" if q == "pool" else "hw"
        ent = self.dma_sems.get((id(sb), qt))
        if ent is None:
            if self.free_sems[qt]:
                ent = self.free_sems[qt].pop()
            else:
                sem = self.stack.enter_context(self.nc.semaphore("ds%d" % self.nsem))
                self.nsem += 1
                ent = [sem, 0, qt]
            self.dma_sems[(id(sb), qt)] = ent
        ent[1] += 16
        op.sem = ent[0]
        op.semval = ent[1]
        return op

    def barrier(self):
        lasts = [self.last_c[e] for e in self.ENGS if self.last_c[e] is not None]
        dmas = self.dmas
        self.dmas = []
        for ent in self.dma_sems.values():
            self.free_sems[ent[2]].append(ent)
        self.dma_sems = {}
        for e in self.ENGS:
            op = Op()
            op.eng = e
            op.fn = None
            op.needed = False
            op.cnt = 0
            op.is_dma = False
            op.sem = None
            op.semval = 0
            op.deps = [d for d in lasts + dmas if d.is_dma or d.eng != e]
            self.ops[e].append(op)

    def emit(self):
        nc = self.nc
        for e in self.ENGS:
            for op in self.ops[e]:
                for d in op.deps:
                    d.needed = True
        for e in self.ENGS:
            c = 0
            for op in self.ops[e]:
                if (not op.is_dma) and op.fn is not None and op.needed:
                    c += 1
                op.cnt = c
        esem = self.esem
        allops = self.ops

        def run(ename, eng):
            known = {}
            for op in allops[ename]:
                for d in op.deps:
                    if d.is_dma:
                        sem, val = d.sem, d.semval
                    else:
                        sem, val = esem[d.eng], d.cnt
                    if known.get(id(sem), 0) < val:
                        eng.wait_ge(sem, val)
                        known[id(sem)] = val
                if op.fn is None:
                    continue
                ins = op.fn(eng)
                if op.is_dma:
                    ins.then_inc(op.sem, 16)
                elif op.needed:
                    ins.then_inc(esem[ename], 1)

        with nc.Block() as block:
            @block.tensor
            def _(e):
                run("pe", e)

            @block.scalar
            def _(e):
                run("act", e)

            @block.vector
            def _(e):
                run("dve", e)

            @block.gpsimd
            def _(e):
                run("pool", e)

            @block.sync
            def _(e):
                run("sp", e)


class StopBuild(Exception):
    pass


class T:
    def __init__(self, h, name):
        self.h = h
        self.b = Buf(name)

    def __getitem__(self, k):
        return self.h[k]


def build_nc(cfg=None):
    cfg = cfg or {}
    n_layers = cfg.get("layers", DEPTH)
    do_moe = cfg.get("moe", True)
    n_exp = cfg.get("n_exp", E)
    tap = cfg.get("tap", None)

    nc = bass.Bass("TRN2", target_bir_lowering=False)

    def din(name, shape, dt=F32):
        return nc.dram_tensor(name, list(shape), dt, kind="ExternalInput").ap()

    def dscr(name, shape, dt=F32):
        return nc.dram_tensor(name, list(shape), dt, kind="Internal").ap()

    xT = din("xT", [D, S])
    cT = din("cT", [128, KC])
    pos = din("pos", [1, S], I32)
    cst = din("cst", [128, 4])
    w_ada = din("w_ada", [DEPTH * D, 6 * D])
    b_adaT = din("b_adaT", [DEPTH * 128, 48])
    n1g = din("n1g", [DEPTH * 128, KC])
    n2g = din("n2g", [DEPTH * 128, KC])
    fg = din("fg", [128, KC])
    w_in = din("w_in", [DEPTH * D, INW])
    w_in_sw = din("w_in_sw", [DEPTH * D, 1536])
    w_sT = din("w_sT", [DEPTH * 128, 512])
    maskT = din("maskT", [128, 512])
    attm = din("attm", [128, 512])
    b_s = din("b_s", [DEPTH, 512])
    ln_g = din("ln_g", [DEPTH, 512])
    ln_b = din("ln_b", [DEPTH, 512])
    w_pa = din("w_pa", [DEPTH * 256, D])
    w_pg = din("w_pg", [DEPTH * 512, D])
    w_o = din("w_o", [DEPTH * D, D])
    w_router = din("w_router", [DEPTH * D, E])
    b_router = din("b_router", [DEPTH, E])
    w1 = din("w1", [DEPTH * E * D if do_moe else 128, 2 * D])
    b1T = din("b1T", [DEPTH * 128, E * 16])
    w2 = din("w2", [DEPTH * E * D if do_moe else 128, D])
    b2 = din("b2", [DEPTH * E, D])
    outT = nc.dram_tensor("outT", [D, S], F32, kind="ExternalOutput").ap()

    hT = dscr("hT", [D, S])
    QT = dscr("QT", [768, S], BF16)
    KT = dscr("KT", [768, S], BF16)
    Vd = dscr("Vd", [S, 768], BF16)
    sigA = dscr("sigA", [D, S])
    part2 = dscr("part2", [D, S])
    cosd = dscr("cosd", [128, S])
    sind = dscr("sind", [128, S])
    dbg = None
    if tap is not None:
        dbg = nc.dram_tensor("dbg", list(cfg["tap_shape"]), F32, kind="ExternalOutput").ap()

    top = ExitStack()
    with top:
        kb = KB(nc, top)

        uniq = [0]

        def sb(stack, name, shape, dt=F32):
            uniq[0] += 1
            name = "%s_%d" % (name, uniq[0])
            return T(stack.enter_context(nc.sbuf_tensor(name, list(shape), dt)), name)

        PS = [T(top.enter_context(nc.psum_tensor("ps%d" % i, [128, 512], F32)), "ps%d" % i) for i in range(8)]

        d_hT = [Buf("hT%d" % i) for i in range(NST)]
        d_QT = Buf("QT")
        d_KT = Buf("KT")
        d_V = Buf("V")
        d_sigA = [Buf("sigA%d" % i) for i in range(NST)]
        d_part2 = [Buf("part2%d" % i) for i in range(NST)]
        d_cos = Buf("cosd")
        d_out = Buf("out")
        d_dbg = Buf("dbg")

        ones_f = sb(top, "ones_f", [128, 128])
        ident = sb(top, "ident", [128, 128])
        cst_t = sb(top, "cst_t", [128, 4])
        eps_t = sb(top, "eps_t", [128, 2])
        modv = sb(top, "modv", [128, 48])
        wv = sb(top, "wv", [128, 16])
        kb.op("dve", lambda e: e.memset(ones_f[:], 1.0), writes=[ones_f.b])
        kb.op("dve", lambda e: e.memset(eps_t[:, 0:1], RMS_EPS), writes=[eps_t.b])
        kb.op("dve", lambda e: e.memset(eps_t[:, 1:2], LN_EPS), writes=[eps_t.b])
        kb.dma("sp", cst_t[:], cst, cst_t.b, writes=[cst_t.b])
        with ExitStack() as ph:
            it = sb(ph, "iota_t", [128, 128], I32)
            kb.op("pool", lambda e: e.iota(it[:], [[1, 128]], base=0, channel_multiplier=-1), writes=[it.b])
            kb.op("dve", lambda e: e.tensor_scalar(ident[:], it[:], 0.0, None, ALU.is_equal), reads=[it.b], writes=[ident.b])
            kb.barrier()

        with ExitStack() as ph:
            posi = sb(ph, "posi", [128, S], I32)
            ang = sb(ph, "ang", [128, S])
            t1 = sb(ph, "rt1", [128, S])
            t2 = sb(ph, "rt2", [128, S])
            ki = sb(ph, "rki", [128, S], I32)
            kb.dma("sp", posi[:], pos.partition_broadcast(128), posi.b, writes=[posi.b])
            kb.op("dve", lambda e: e.tensor_copy(ang[:], posi[:]), reads=[posi.b], writes=[ang.b])
            kb.op("dve", lambda e: e.tensor_scalar(ang[:], ang[:], cst_t[:, 0:1], None, ALU.mult),
                  reads=[ang.b, cst_t.b], writes=[ang.b])
            for which, dst in ((0, sind), (1, cosd)):
                shift = 0.0 if which == 0 else math.pi / 2.0
                kb.op("dve", lambda e, sh=shift: e.tensor_scalar(t1[:], ang[:], sh, None, ALU.add),
                      reads=[ang.b], writes=[t1.b])
                kb.op("dve", lambda e: e.tensor_scalar(t2[:], t1[:], 1.0 / TWO_PI, 0.5, ALU.mult, ALU.add),
                      reads=[t1.b], writes=[t2.b])
                kb.op("dve", lambda e: e.tensor_copy(ki[:], t2[:]), reads=[t2.b], writes=[ki.b])
                kb.op("dve", lambda e: e.tensor_copy(t2[:], ki[:]), reads=[ki.b], writes=[t2.b])
                c_hi = float(np.float32(TWO_PI))
                c_lo = float(TWO_PI - np.float64(np.float32(TWO_PI)))
                kb.op("dve", lambda e: e.scalar_tensor_tensor(t1[:], t2[:], -c_hi, t1[:], ALU.mult, ALU.add),
                      reads=[t1.b, t2.b], writes=[t1.b])
                kb.op("dve", lambda e: e.scalar_tensor_tensor(t1[:], t2[:], -c_lo, t1[:], ALU.mult, ALU.add),
                      reads=[t1.b, t2.b], writes=[t1.b])
                kb.op("dve", lambda e: e.tensor_scalar(t2[:], t1[:], -math.pi, TWO_PI, ALU.is_lt, ALU.mult),
                      reads=[t1.b], writes=[t2.b])
                kb.op("dve", lambda e: e.tensor_tensor(t1[:], t1[:], t2[:], ALU.add), reads=[t1.b, t2.b], writes=[t1.b])
                kb.op("dve", lambda e: e.tensor_scalar(t2[:], t1[:], math.pi, -TWO_PI, ALU.is_gt, ALU.mult),
                      reads=[t1.b], writes=[t2.b])
                kb.op("dve", lambda e: e.tensor_tensor(t1[:], t1[:], t2[:], ALU.add), reads=[t1.b, t2.b], writes=[t1.b])
                kb.op("dve", lambda e: e.tensor_scalar(t1[:], t1[:], 3.1415925, -3.1415925, ALU.min, ALU.max),
                      reads=[t1.b], writes=[t1.b])
                kb.op("act", lambda e: e.activation(t2[:], t1[:], AF.Sin), reads=[t1.b], writes=[t2.b])
                if which == 0:
                    kb.op("dve", lambda e: e.tensor_scalar(t2[:], t2[:], cst_t[:, 1:2], None, ALU.mult),
                          reads=[t2.b, cst_t.b], writes=[t2.b])
                kb.dma("sp", dst, t2[:], t2.b, reads=[t2.b], writes=[d_cos])
            kb.barrier()

        if tap == "cos":
            with ExitStack() as ph:
                tt = sb(ph, "tapt", [128, S])
                kb.dma("sp", tt[:], cosd, tt.b, reads=[d_cos], writes=[tt.b])
                kb.dma("sp", dbg[0:128, :], tt[:], tt.b, reads=[tt.b], writes=[d_dbg])
                kb.dma("sp", tt[:], sind, tt.b, reads=[d_cos, d_dbg], writes=[tt.b])
                kb.dma("sp", dbg[128:256, :], tt[:], tt.b, reads=[tt.b], writes=[d_dbg])
                kb.barrier()

        def rmsnorm_to(ph_tiles, h_t, wv_cols, sh_cols, a_bf, a_f32=None, ps_idx=7, a_off=0):
            sq, rstd = ph_tiles
            ps = PS[ps_idx]
            kb.op("act", lambda e: e.activation(sq[:], h_t[:], AF.Square), reads=[h_t.b], writes=[sq.b])
            for kc in range(KC):
                kb.op("pe", lambda e, kc=kc: e.matmul(ps[:], ones_f[:], sq[:, kc, :], start=(kc == 0), stop=(kc == KC - 1)),
                      reads=[ones_f.b, sq.b], writes=[ps.b])
            kb.op("act", lambda e: e.activation(rstd[:], ps[:], AF.Sqrt, bias=eps_t[:, 0:1], scale=1.0 / D),
                  reads=[ps.b, eps_t.b], writes=[rstd.b])
            kb.op("dve", lambda e: e.reciprocal(rstd[:], rstd[:]), reads=[rstd.b], writes=[rstd.b])
            for kc in range(KC):
                kb.op("dve", lambda e, kc=kc: e.tensor_tensor(sq[:, kc, :], h_t[:, kc, :], rstd[:], ALU.mult),
                      reads=[h_t.b, rstd.b], writes=[sq.b])
                kb.op("act", lambda e, kc=kc: e.activation(a_bf[:, kc, a_off:a_off + ST], sq[:, kc, :], AF.Identity,
                                                            bias=modv[:, sh_cols + kc:sh_cols + kc + 1],
                                                            scale=wv[:, wv_cols + kc:wv_cols + kc + 1]),
                      reads=[sq.b, modv.b, wv.b], writes=[a_bf.b])
                if a_f32 is not None:
                    kb.op("act", lambda e, kc=kc: e.activation(a_f32[:, kc, :], sq[:, kc, :], AF.Identity,
                                                                bias=modv[:, sh_cols + kc:sh_cols + kc + 1],
                                                                scale=wv[:, wv_cols + kc:wv_cols + kc + 1]),
                          reads=[sq.b, modv.b, wv.b], writes=[a_f32.b])

        stopf = [False]

        def one_layer(l):
          if True:
              src_h = xT if l == 0 else hT
              phw = ExitStack()
              win = sb(phw, "win", [128, KC, INW], BF16)
              wsw = sb(phw, "wsw", [128, KC, 1536], BF16)
              wpg = sb(phw, "wpg", [128, 4, D], BF16)
              winv = w_in[l * D:(l + 1) * D, :].rearrange("(kc p) n -> p kc n", p=128)
              wswv = w_in_sw[l * D:(l + 1) * D, :].rearrange("(kc p) n -> p kc n", p=128)
              for c0 in range(0, INW, 1792):
                  for kc in range(KC):
                      kb.dma("pool", win[:, kc, c0:c0 + 1792], winv[:, kc, c0:c0 + 1792], win.b, writes=[win.b])
                  if c0 == 0:
                      for kc in range(KC):
                          kb.dma("pool", wsw[:, kc, :], wswv[:, kc, :], wsw.b, writes=[wsw.b])
              wpgv = w_pg[l * 512:(l + 1) * 512, :].rearrange("(kc p) n -> p kc n", p=128)
              for kc in range(4):
                  kb.dma("pool", wpg[:, kc, :], wpgv[:, kc, :], wpg.b, writes=[wpg.b])
              with ExitStack() as ph:
                  ct = sb(ph, "ct", [128, KC])
                  sc = sb(ph, "sc", [128, KC])
                  bad = sb(ph, "bad", [128, 48])
                  ng = sb(ph, "ng", [128, 16])
                  wa = [sb(ph, "wa%d" % i, [128, KC, 512]) for i in range(2)]
                  kb.dma("sp", ct[:], cT, ct.b, writes=[ct.b])
                  kb.dma("sp", bad[:], b_adaT[l * 128:(l + 1) * 128, :], bad.b, writes=[bad.b])
                  kb.dma("sp", ng[:, 0:8], n1g[l * 128:(l + 1) * 128, :], ng.b, writes=[ng.b])
                  kb.dma("sp", ng[:, 8:16], n2g[l * 128:(l + 1) * 128, :], ng.b, writes=[ng.b])
                  kb.op("act", lambda e: e.activation(sc[:], ct[:], AF.Silu), reads=[ct.b], writes=[sc.b])
                  ps = PS[0]
                  wav = w_ada[l * D:(l + 1) * D, :].rearrange("(kc p) n -> p kc n", p=128)
                  for cc in range(12):
                      wt = wa[cc % 2]
                      kb.dma("sp", wt[:], wav[:, :, cc * 512:(cc + 1) * 512], wt.b, writes=[wt.b])
                      for j in range(4):
                          col = cc * 4 + j
                          for kc in range(KC):
                              kb.op("pe", lambda e, wt=wt, j=j, kc=kc, col=col: e.matmul(
                                  ps[:, col:col + 1], wt[:, kc, j * 128:(j + 1) * 128], sc[:, kc:kc + 1],
                                  start=(kc == 0), stop=(kc == KC - 1)),
                                  reads=[wt.b, sc.b], writes=[ps.b])
                  kb.op("dve", lambda e: e.tensor_tensor(modv[:], ps[:, 0:48], bad[:], ALU.add),
                        reads=[ps.b, bad.b], writes=[modv.b])
                  kb.op("dve", lambda e: e.scalar_tensor_tensor(wv[:, 0:8], modv[:, 8:16], 1.0, ng[:, 0:8], ALU.add, ALU.mult),
                        reads=[modv.b, ng.b], writes=[wv.b])
                  kb.op("dve", lambda e: e.scalar_tensor_tensor(wv[:, 8:16], modv[:, 32:40], 1.0, ng[:, 8:16], ALU.add, ALU.mult),
                        reads=[modv.b, ng.b], writes=[wv.b])
                  kb.barrier()

              if tap == "mod" and l == cfg.get("tap_layer", 0):
                  kb.dma("sp", dbg[:, 0:48], modv[:], modv.b, reads=[modv.b], writes=[d_dbg])
                  kb.dma("sp", dbg[:, 48:64], wv[:], wv.b, reads=[wv.b], writes=[d_dbg])
                  kb.barrier()
                  return

              with ExitStack() as ph:
                  wst = sb(ph, "wst", [128, 512], BF16)
                  wsf = sb(ph, "wsf", [128, 512])
                  mkf = sb(ph, "mkf", [128, 512])
                  lng = sb(ph, "lng", [128, 512])
                  lnb = sb(ph, "lnb", [128, 512])
                  bsr = sb(ph, "bsr", [1, 512])
                  h_t = [sb(ph, "h_t%d" % i, [128, KC, ST]) for i in range(1)]
                  sq = sb(ph, "sq", [128, KC, ST])
                  rstd = sb(ph, "rstd", [128, ST])
                  a_bf = sb(ph, "a_bf", [128, KC, ST], BF16)
                  cs = [sb(ph, "cs%d" % i, [128, 2, ST]) for i in range(2)]
                  r1 = sb(ph, "r1", [128, ST])
                  r2 = sb(ph, "r2", [128, ST])
                  qo = [sb(ph, "qo%d" % i, [128, ST], BF16) for i in range(2)]
                  vo = [sb(ph, "vo%d" % i, [128, 768], BF16) for i in range(2)]
                  g1t = sb(ph, "g1t", [128, ST])
                  g2t = sb(ph, "g2t", [128, ST])
                  g3t = sb(ph, "g3t", [128, ST])
                  vgn2 = [sb(ph, "vgn%d" % i, [128, 512], BF16) for i in range(2)]
                  st1 = sb(ph, "st1", [128, 4])
                  gmT = sb(ph, "gmT", [128, 4, ST], BF16)
                  so = [sb(ph, "so%d" % i, [128, ST]) for i in range(1)]
                  po = [sb(ph, "po%d" % i, [128, ST]) for i in range(1)]

                  kb.dma("sp", wsf[:], w_sT[l * 128:(l + 1) * 128, :], wsf.b, writes=[wsf.b])
                  kb.dma("sp", mkf[:], maskT, mkf.b, writes=[mkf.b])
                  kb.op("dve", lambda e: e.tensor_tensor(wst[:], wsf[:], mkf[:], ALU.mult), reads=[wsf.b, mkf.b], writes=[wst.b])
                  kb.dma("sp", lng[:], ln_g[l:l + 1, :].partition_broadcast(128), lng.b, writes=[lng.b])
                  kb.dma("sp", lnb[:], ln_b[l:l + 1, :].partition_broadcast(128), lnb.b, writes=[lnb.b])
                  kb.dma("sp", bsr[:], b_s[l:l + 1, :], bsr.b, writes=[bsr.b])

                  pscnt = [0]

                  def nps():
                      p = PS[pscnt[0] % 6]
                      pscnt[0] += 1
                      return p

                  def proj_fm(wt, c0, rhs_t):
                      p = nps()
                      for kc in range(KC):
                          kb.op("pe", lambda e, kc=kc: e.matmul(p[:], wt[:, kc, c0:c0 + 128], rhs_t[:, kc, :],
                                                                  start=(kc == 0), stop=(kc == KC - 1)),
                                reads=[wt.b, rhs_t.b], writes=[p.b])
                      return p

                  def gelu_from(p_ap, pbuf, out_ap, obuf, shape_t):
                      a1, a2 = shape_t
                      kb.op("act", lambda e: e.activation(a1, p_ap, AF.Square), reads=[pbuf], writes=[g1t.b])
                      kb.op("dve", lambda e: e.tensor_scalar(a1, a1, 0.044715, 1.0, ALU.mult, ALU.add),
                            reads=[g1t.b], writes=[g1t.b])
                      kb.op("dve", lambda e: e.tensor_tensor(a1, a1, p_ap, ALU.mult), reads=[g1t.b, pbuf], writes=[g1t.b])
                      kb.op("act", lambda e: e.activation(a2, a1, AF.Sigmoid, scale=2.0 * math.sqrt(2.0 / math.pi)),
                            reads=[g1t.b], writes=[g2t.b])
                      kb.op("dve", lambda e: e.tensor_tensor(out_ap, a2, p_ap, ALU.mult), reads=[g2t.b, pbuf], writes=[obuf])

                  def load_a(s_):
                      t0_ = s_ * ST
                      ht_ = h_t[0]
                      c_ = cs[s_ % 2]
                      kb.dma("sp", ht_[:], src_h[:, t0_:t0_ + ST].rearrange("(kc p) t -> p kc t", p=128), ht_.b,
                             reads=[d_hT[s_]], writes=[ht_.b])
                      kb.dma("sp", c_[:, 0, :], cosd[:, t0_:t0_ + ST], c_.b, reads=[d_cos], writes=[c_.b])
                      kb.dma("sp", c_[:, 1, :], sind[:, t0_:t0_ + ST], c_.b, reads=[d_cos], writes=[c_.b])

                  for s in range(NST):
                      t0 = s * ST
                      ht = h_t[0]
                      cst2 = cs[s % 2]
                      if s == 0:
                          load_a(0)
                      rmsnorm_to((sq, rstd), ht, 0, 0, a_bf)
                      if s + 1 < NST:
                          load_a(s + 1)
                      if tap == "a" and l == cfg.get("tap_layer", 0) and s == 0:
                          for kc in range(KC):
                              kb.op("dve", lambda e, kc=kc: e.tensor_copy(sq[:, kc, :], a_bf[:, kc, :]), reads=[a_bf.b], writes=[sq.b])
                          kb.dma("sp", dbg.rearrange("(kc p) t -> p kc t", p=128), sq[:], sq.b, reads=[sq.b], writes=[d_dbg])
                          stopf[0] = True
                          break
                      for qk in range(2):
                          dst, dbuf = (QT, d_QT) if qk == 0 else (KT, d_KT)
                          for ch in range(6):
                              c0 = qk * 768 + ch * 128
                              p1 = proj_fm(win, c0, a_bf)
                              p2 = proj_fm(wsw, c0, a_bf)
                              o = qo[(qk * 6 + ch) % 2]
                              kb.op("dve", lambda e, p1=p1, cst2=cst2: e.tensor_tensor(r1[:], p1[:], cst2[:, 0, :], ALU.mult),
                                    reads=[p1.b, cst2.b], writes=[r1.b])
                              kb.op("dve", lambda e, p2=p2, cst2=cst2: e.tensor_tensor(r2[:], p2[:], cst2[:, 1, :], ALU.mult),
                                    reads=[p2.b, cst2.b], writes=[r2.b])
                              kb.op("pool", lambda e, o=o: e.tensor_tensor(o[:], r1[:], r2[:], ALU.add),
                                    reads=[r1.b, r2.b], writes=[o.b])
                              kb.dma("sp", dst[ch * 128:(ch + 1) * 128, t0:t0 + ST], o[:], o.b, reads=[o.b], writes=[dbuf])
                      def do_mix(tk0, vgn):
                          pm = nps()
                          for g in range(4):
                              kb.op("pe", lambda e, g=g: e.matmul(pm[:, g * 128:(g + 1) * 128], vgn[:, g * 128:(g + 1) * 128],
                                                                   wst[:, g * 128:(g + 1) * 128], start=True, stop=False),
                                    reads=[vgn.b, wst.b], writes=[pm.b])
                              kb.op("pe", lambda e, g=g: e.matmul(pm[:, g * 128:(g + 1) * 128], ones_f[0:1, :],
                                                                   bsr[:, g * 128:(g + 1) * 128], start=False, stop=True),
                                    reads=[ones_f.b, bsr.b], writes=[pm.b])
                          kb.op("act", lambda e: e.copy(sq[:, 0:4, tk0:tk0 + 128], pm[:].rearrange("p (g t) -> p g t", g=4)),
                                reads=[pm.b], writes=[sq.b])

                      prev_mix = None
                      for tt in range(4):
                          tk0 = tt * 128
                          v_o = vo[tt % 2]
                          vgn = vgn2[tt % 2]
                          for (c0, n) in ((1536, 512), (2048, 256)):
                              p = nps()
                              for kc in range(KC):
                                  kb.op("pe", lambda e, kc=kc, p=p, c0=c0, n=n, tk0=tk0: e.matmul(
                                      p[:, 0:n], a_bf[:, kc, tk0:tk0 + 128], win[:, kc, c0:c0 + n],
                                      start=(kc == 0), stop=(kc == KC - 1)),
                                      reads=[a_bf.b, win.b], writes=[p.b])
                              kb.op("act", lambda e, p=p, c0=c0, n=n, v_o=v_o: e.copy(v_o[:, c0 - 1536:c0 - 1536 + n], p[:, 0:n]),
                                    reads=[p.b], writes=[v_o.b])
                          kb.dma("sp", Vd[t0 + tk0:t0 + tk0 + 128, :], v_o[:], v_o.b, reads=[v_o.b], writes=[d_V])
                          p = nps()
                          for kc in range(KC):
                              kb.op("pe", lambda e, kc=kc, p=p, tk0=tk0: e.matmul(
                                  p[:], a_bf[:, kc, tk0:tk0 + 128], win[:, kc, 2816:3328],
                                  start=(kc == 0), stop=(kc == KC - 1)),
                                  reads=[a_bf.b, win.b], writes=[p.b])
                          gelu_from(p[:], p.b, g3t[:], g3t.b, (g1t[:], g2t[:]))
                          kb.op("dve", lambda e: e.reduce_sum(st1[:, 0:1], g3t[:], AX.X), reads=[g3t.b], writes=[st1.b])
                          kb.op("dve", lambda e: e.tensor_scalar(st1[:, 1:2], st1[:, 0:1], -1.0 / 512, None, ALU.mult),
                                reads=[st1.b], writes=[st1.b])
                          kb.op("act", lambda e: e.activation(g1t[:], g3t[:], AF.Identity, bias=st1[:, 1:2]),
                                reads=[g3t.b, st1.b], writes=[g1t.b])
                          kb.op("act", lambda e: e.activation(g2t[:], g1t[:], AF.Square, accum_out=st1[:, 2:3]),
                                reads=[g1t.b], writes=[g2t.b, st1.b])
                          kb.op("act", lambda e: e.activation(st1[:, 3:4], st1[:, 2:3], AF.Sqrt, bias=eps_t[:, 1:2], scale=1.0 / 512),
                                reads=[st1.b, eps_t.b], writes=[st1.b])
                          kb.op("dve", lambda e: e.reciprocal(st1[:, 3:4], st1[:, 3:4]), reads=[st1.b], writes=[st1.b])
                          kb.op("dve", lambda e: e.scalar_tensor_tensor(g2t[:], g1t[:], st1[:, 3:4], lng[:], ALU.mult, ALU.mult),
                                reads=[g1t.b, st1.b, lng.b], writes=[g2t.b])
                          kb.op("dve", lambda e, vgn=vgn: e.tensor_tensor(vgn[:], g2t[:], lnb[:], ALU.add),
                                reads=[g2t.b, lnb.b], writes=[vgn.b])
                          if prev_mix is not None:
                              do_mix(*prev_mix)
                          prev_mix = (tk0, vgn)
                      do_mix(*prev_mix)
                      for g in range(4):
                          p = proj_fm(win, 2304 + g * 128, a_bf)
                          gelu_from(p[:], p.b, g3t[:], g3t.b, (g1t[:], g2t[:]))
                          kb.op("dve", lambda e, g=g: e.tensor_tensor(gmT[:, g, :], g3t[:], sq[:, g, :], ALU.mult),
                                reads=[g3t.b, sq.b], writes=[gmT.b])
                      if tap == "gm" and l == cfg.get("tap_layer", 0) and s == 0:
                          for g in range(4):
                              kb.op("dve", lambda e, g=g: e.tensor_copy(sq[:, 4 + g, :], gmT[:, g, :]), reads=[gmT.b], writes=[sq.b])
                          kb.dma("sp", dbg.rearrange("(kc p) t -> p kc t", p=128), sq[:, 4:8, :], sq.b, reads=[sq.b], writes=[d_dbg])
                          stopf[0] = True
                          break
                      for dc in range(KC):
                          p = proj_fm(win, 3328 + dc * 128, a_bf)
                          o = so[0]
                          kb.op("act", lambda e, p=p, o=o: e.activation(o[:], p[:], AF.Sigmoid), reads=[p.b], writes=[o.b])
                          kb.dma("sp", sigA[dc * 128:(dc + 1) * 128, t0:t0 + ST], o[:], o.b, reads=[o.b], writes=[d_sigA[s]])
                          p = proj_fm(win, 4352 + dc * 128, a_bf)
                          kb.op("act", lambda e, p=p: e.activation(r1[:], p[:], AF.Sigmoid), reads=[p.b], writes=[r1.b])
                          p2 = nps()
                          for kc in range(4):
                              kb.op("pe", lambda e, kc=kc, p2=p2, dc=dc: e.matmul(
                                  p2[:], wpg[:, kc, dc * 128:(dc + 1) * 128], gmT[:, kc, :], start=(kc == 0), stop=(kc == 3)),
                                  reads=[wpg.b, gmT.b], writes=[p2.b])
                          o2 = po[0]
                          kb.op("dve", lambda e, p2=p2, o2=o2: e.tensor_tensor(o2[:], p2[:], r1[:], ALU.mult),
                                reads=[p2.b, r1.b], writes=[o2.b])
                          kb.dma("sp", part2[dc * 128:(dc + 1) * 128, t0:t0 + ST], o2[:], o2.b, reads=[o2.b], writes=[d_part2[s]])
                  kb.barrier()

              phw.close()
              if stopf[0]:
                  return
              if tap == "pa" and l == cfg.get("tap_layer", 0):
                  with ExitStack() as ph:
                      tt = sb(ph, "tapt", [128, KC, ST])
                      for s in range(NST):
                          kb.dma("sp", tt[:], part2[:, s * ST:(s + 1) * ST].rearrange("(kc p) t -> p kc t", p=128), tt.b,
                                 reads=[d_part2[s], d_dbg], writes=[tt.b])
                          kb.dma("sp", dbg[:, s * ST:(s + 1) * ST].rearrange("(kc p) t -> p kc t", p=128), tt[:], tt.b,
                                 reads=[tt.b], writes=[d_dbg])
                      kb.barrier()
                  return
              with ExitStack() as ph:
                  numT = sb(ph, "numT", [128, 2, S])
                  denT = sb(ph, "denT", [128, 2, S])
                  pha = ExitStack()
                  qc = [sb(pha, "qc%d" % i, [128, S], BF16) for i in range(2)]
                  kz = [[sb(pha, "kz%d_%d" % (i, hh), [128, S], BF16) for hh in range(2)] for i in range(2)]
                  vz = [sb(pha, "vz%d" % i, [128, 2, 128], BF16) for i in range(5)]
                  onz = sb(pha, "onz", [128, 2, 128], BF16)
                  am = sb(pha, "am", [128, 512], BF16)
                  amf = sb(pha, "amf", [128, 512])
                  pT = [sb(pha, "pT%d" % i, [128, 256], BF16) for i in range(6)]
                  for v in vz:
                      kb.op("pool", lambda e, v=v: e.memset(v[:], 0.0), writes=[v.b])
                  for i in range(2):
                      for hh in range(2):
                          kb.op("pool", lambda e, i=i, hh=hh: e.memset(kz[i][hh][:], 0.0), writes=[kz[i][hh].b])
                  kb.op("pool", lambda e: e.memset(onz[:], 0.0), writes=[onz.b])
                  kb.op("pool", lambda e: e.memset(onz[:, 0, 0:64], 1.0), writes=[onz.b])
                  kb.op("pool", lambda e: e.memset(onz[:, 1, 64:128], 1.0), writes=[onz.b])
                  kb.dma("sp", amf[:], attm, amf.b, writes=[amf.b])
                  kb.op("dve", lambda e: e.tensor_copy(am[:], amf[:]), reads=[amf.b], writes=[am.b])
                  kb.barrier()

                  att_groups = cfg.get("att_groups", [0, 1, 2])
                  cnts = {"it": 0, "v": 0, "p": 0}

                  def stage1(g, r, c, q_t, k_t, f0, j, n, vprev):
                      tok0 = n * 128 * r + j
                      v_t = vz[cnts["v"] % 5]
                      cnts["v"] += 1
                      vsrc = Vd[tok0:tok0 + 127 * r + 1:r, f0:f0 + 128].rearrange("k (h d) -> k h d", h=2)
                      for hh in range(2):
                          kb.dma("sp", v_t[:, hh, hh * 64:(hh + 1) * 64], vsrc[:, hh, :], v_t.b, reads=[d_V], writes=[v_t.b])
                      qsl = slice(tok0, tok0 + 127 * r + 1, r)
                      kbs = []
                      if n > 0:
                          kbs.append((slice(tok0 - 128 * r, tok0 - r + 1, r), vprev, 0))
                      kbs.append((qsl, v_t, 1))
                      pts = []
                      for (ksl, vt_, mi) in kbs:
                          pss = PS[cnts["p"] % 4]
                          p_t = pT[cnts["p"] % 6]
                          cnts["p"] += 1
                          for hh in range(2):
                              kb.op("pe", lambda e, hh=hh, pss=pss, ksl=ksl: e.matmul(
                                  pss[:, hh * 128:(hh + 1) * 128], k_t[hh][:, ksl], q_t[:, qsl], start=True, stop=True),
                                  reads=[k_t[hh].b, q_t.b], writes=[pss.b])
                          kb.op("act", lambda e, pss=pss, p_t=p_t: e.activation(p_t[:], pss[:, 0:256], AF.Exp, scale=0.125),
                                reads=[pss.b], writes=[p_t.b])
                          kb.op("pool", lambda e, p_t=p_t, mi=mi: e.tensor_tensor(p_t[:], p_t[:], am[:, mi * 256:(mi + 1) * 256], ALU.mult),
                                reads=[p_t.b, am.b], writes=[p_t.b])
                          pts.append((p_t, vt_))
                      return (g, c, qsl, pts), v_t

                  def stage2(unit):
                      g, c, qsl, pts = unit
                      po_ = PS[4 + (cnts["it"] % 2)]
                      pd_ = PS[6 + (cnts["it"] % 2)]
                      cnts["it"] += 1
                      for bi, (p_t, vt_) in enumerate(pts):
                          first = (bi == 0)
                          last = (bi == len(pts) - 1)
                          for hh in range(2):
                              kb.op("pe", lambda e, hh=hh, vt_=vt_, p_t=p_t, first=first, last=last: e.matmul(
                                  po_[:, 0:128], vt_[:, hh, :], p_t[:, hh * 128:(hh + 1) * 128],
                                  start=(first and hh == 0), stop=(last and hh == 1)),
                                  reads=[vt_.b, p_t.b], writes=[po_.b])
                          for hh in range(2):
                              kb.op("pe", lambda e, hh=hh, p_t=p_t, first=first, last=last: e.matmul(
                                  pd_[:, 0:128], onz[:, hh, :], p_t[:, hh * 128:(hh + 1) * 128],
                                  start=(first and hh == 0), stop=(last and hh == 1)),
                                  reads=[onz.b, p_t.b], writes=[pd_.b])
                      if g == att_groups[0]:
                          kb.op("dve", lambda e: e.tensor_copy(numT[:, c, qsl], po_[:, 0:128]), reads=[po_.b], writes=[numT.b])
                          kb.op("dve", lambda e: e.tensor_copy(denT[:, c, qsl], pd_[:, 0:128]), reads=[pd_.b], writes=[denT.b])
                      else:
                          kb.op("dve", lambda e: e.tensor_tensor(numT[:, c, qsl], po_[:, 0:128], numT[:, c, qsl], ALU.add),
                                reads=[po_.b, numT.b], writes=[numT.b])
                          kb.op("dve", lambda e: e.tensor_tensor(denT[:, c, qsl], pd_[:, 0:128], denT[:, c, qsl], ALU.add),
                                reads=[pd_.b, denT.b], writes=[denT.b])

                  pending = None
                  for g, (win_, r) in enumerate(GROUPS):
                      if g not in att_groups:
                          continue
                      L = S // r
                      nb = L // 128
                      for c in range(2):
                          q_t = qc[(g * 2 + c) % 2]
                          k_t = kz[(g * 2 + c) % 2]
                          f0 = g * 256 + c * 128
                          kb.dma("sp", q_t[:], QT[f0:f0 + 128, :], q_t.b, reads=[d_QT], writes=[q_t.b])
                          for hh in range(2):
                              kb.dma("sp", k_t[hh][hh * 64:(hh + 1) * 64, :], KT[f0 + hh * 64:f0 + (hh + 1) * 64, :], k_t[hh].b,
                                     reads=[d_KT], writes=[k_t[hh].b])
                          for j in range(r):
                              vprev = None
                              for n in range(min(nb, cfg.get("att_nblk", 10 ** 6))):
                                  unit, vprev = stage1(g, r, c, q_t, k_t, f0, j, n, vprev)
                                  if pending is not None:
                                      stage2(pending)
                                  pending = unit
                  if pending is not None:
                      stage2(pending)
                  kb.barrier()
                  pha.close()

                  if tap == "attn" and l == cfg.get("tap_layer", 0):
                      kb.dma("sp", dbg[0:256, :].rearrange("(c p) t -> p c t", p=128), numT[:], numT.b, reads=[numT.b], writes=[d_dbg])
                      kb.dma("sp", dbg[256:512, :].rearrange("(c p) t -> p c t", p=128), denT[:], denT.b, reads=[denT.b], writes=[d_dbg])
                      kb.barrier()
                      stopf[0] = True

                  with ExitStack() as ph2:
                      wpa = sb(ph2, "wpa", [128, 2, D], BF16)
                      wo = sb(ph2, "wo", [128, KC, D], BF16)
                      attn = sb(ph2, "attn", [128, 2, ST], BF16)
                      sgt = [sb(ph2, "sgt%d" % i, [128, KC, ST]) for i in range(2)]
                      p2t = [sb(ph2, "p2t%d" % i, [128, KC, ST]) for i in range(2)]
                      hh_t = [sb(ph2, "hh_t%d" % i, [128, KC, ST]) for i in range(2)]
                      mg = sb(ph2, "mg", [128, KC, ST], BF16)
                      tm = sb(ph2, "tm", [128, ST])
                      wpav = w_pa[l * 256:(l + 1) * 256, :].rearrange("(kc p) n -> p kc n", p=128)
                      for kc in range(2):
                          kb.dma("pool", wpa[:, kc, :], wpav[:, kc, :], wpa.b, writes=[wpa.b])
                      wov = w_o[l * D:(l + 1) * D, :].rearrange("(kc p) n -> p kc n", p=128)
                      for kc in range(KC):
                          kb.dma("pool", wo[:, kc, :], wov[:, kc, :], wo.b, writes=[wo.b])
                      def load_b2(s_):
                          t0_ = s_ * ST
                          kb.dma("sp", sgt[s_ % 2][:], sigA[:, t0_:t0_ + ST].rearrange("(kc p) t -> p kc t", p=128), sgt[s_ % 2].b,
                                 reads=[d_sigA[s_]], writes=[sgt[s_ % 2].b])
                          kb.dma("act", p2t[s_ % 2][:], part2[:, t0_:t0_ + ST].rearrange("(kc p) t -> p kc t", p=128), p2t[s_ % 2].b,
                                 reads=[d_part2[s_]], writes=[p2t[s_ % 2].b])
                          kb.dma("sp", hh_t[s_ % 2][:], src_h[:, t0_:t0_ + ST].rearrange("(kc p) t -> p kc t", p=128), hh_t[s_ % 2].b,
                                 reads=[d_hT[s_]], writes=[hh_t[s_ % 2].b])

                      for s in (range(NST) if not stopf[0] else []):
                          t0 = s * ST
                          sg = sgt[s % 2]
                          p2 = p2t[s % 2]
                          hh2 = hh_t[s % 2]
                          if s == 0:
                              load_b2(0)
                          if s + 1 < NST:
                              load_b2(s + 1)
                          for c in range(2):
                              kb.op("dve", lambda e, c=c, t0=t0: e.reciprocal(tm[:], denT[:, c, t0:t0 + ST]), reads=[denT.b], writes=[tm.b])
                              kb.op("dve", lambda e, c=c, t0=t0: e.tensor_tensor(attn[:, c, :], numT[:, c, t0:t0 + ST], tm[:], ALU.mult),
                                    reads=[numT.b, tm.b], writes=[attn.b])
                          for dc in range(KC):
                              p = PS[dc % 6]
                              for c in range(2):
                                  kb.op("pe", lambda e, c=c, p=p, dc=dc: e.matmul(p[:], wpa[:, c, dc * 128:(dc + 1) * 128], attn[:, c, :],
                                                                                   start=(c == 0), stop=(c == 1)),
                                        reads=[wpa.b, attn.b], writes=[p.b])
                              kb.op("dve", lambda e, p=p, dc=dc, sg=sg: e.tensor_tensor(tm[:], p[:], sg[:, dc, :], ALU.mult),
                                    reads=[p.b, sg.b], writes=[tm.b])
                              kb.op("pool", lambda e, dc=dc, p2=p2: e.tensor_tensor(mg[:, dc, :], tm[:], p2[:, dc, :], ALU.add),
                                    reads=[tm.b, p2.b], writes=[mg.b])
                          for dc in range(KC):
                              p = PS[dc % 6]
                              for kc in range(KC):
                                  kb.op("pe", lambda e, kc=kc, p=p, dc=dc: e.matmul(p[:], wo[:, kc, dc * 128:(dc + 1) * 128], mg[:, kc, :],
                                                                                     start=(kc == 0), stop=(kc == KC - 1)),
                                        reads=[wo.b, mg.b], writes=[p.b])
                              kb.op("dve", lambda e, p=p, dc=dc, hh2=hh2: e.scalar_tensor_tensor(hh2[:, dc, :], p[:], modv[:, 16 + dc:17 + dc],
                                                                                         hh2[:, dc, :], ALU.mult, ALU.add),
                                    reads=[p.b, modv.b, hh2.b], writes=[hh2.b])
                          kb.dma("sp", hT[:, t0:t0 + ST].rearrange("(kc p) t -> p kc t", p=128), hh2[:], hh2.b,
                                 reads=[hh2.b], writes=[d_hT[s]])
                      kb.barrier()

              if stopf[0]:
                  return
              if tap == "h1" and l == cfg.get("tap_layer", 0):
                  with ExitStack() as ph:
                      tt = sb(ph, "tapt", [128, KC, ST])
                      for s in range(NST):
                          kb.dma("sp", tt[:], hT[:, s * ST:(s + 1) * ST].rearrange("(kc p) t -> p kc t", p=128), tt.b,
                                 reads=[d_hT[s], d_dbg], writes=[tt.b])
                          kb.dma("sp", dbg[:, s * ST:(s + 1) * ST].rearrange("(kc p) t -> p kc t", p=128), tt[:], tt.b,
                                 reads=[tt.b], writes=[d_dbg])
                      kb.barrier()
                  return

              if do_moe:
                  with ExitStack() as ph:
                      SL = 1024
                      hb = sb(ph, "hb", [128, KC, ST])
                      sqc = sb(ph, "sq2", [128, KC, ST])
                      rstdc = sb(ph, "rstd2", [128, ST])
                      fT = sb(ph, "fT", [128, KC, SL], BF16)
                      yacc = sb(ph, "yacc", [128, KC, SL])
                      w1p = [sb(ph, "w1p%d" % i, [128, KC, 512], BF16) for i in range(3)]
                      w2e = [sb(ph, "w2e%d" % i, [128, KC, D], BF16) for i in range(2)]
                      actT = [sb(ph, "actT%d" % i, [128, KC, ST], BF16) for i in range(2)]
                      b1t = sb(ph, "b1t", [128, E * 16])
                      b2t = sb(ph, "b2t", [32, D])
                      wr = sb(ph, "wr", [128, KC, E])
                      brt = sb(ph, "brt", [128, E])
                      GT = sb(ph, "GT", [32, SL])
                      sel = sb(ph, "sel", [32, E * 128], BF16)
                      GTh = sb(ph, "GTh", [32, SL], BF16)
                      GTl = sb(ph, "GTl", [32, SL], BF16)
                      GTf = sb(ph, "GTf", [32, SL])
                      lg = sb(ph, "lg", [128, E])
                      mx8 = sb(ph, "mx8", [128, 8])
                      ex = sb(ph, "ex", [128, E])
                      mk = sb(ph, "mk", [128, E])
                      sm = sb(ph, "sm", [128, 2])
                      xg = [sb(ph, "xg%d" % i, [128, ST]) for i in range(2)]
                      sgm = [sb(ph, "sgm%d" % i, [128, ST]) for i in range(2)]
                      xl = [sb(ph, "xl%d" % i, [128, ST]) for i in range(2)]

                      kb.dma("sp", b1t[:], b1T[l * 128:(l + 1) * 128, :], b1t.b, writes=[b1t.b])
                      kb.dma("sp", b2t[:], b2[l * E:(l + 1) * E, :], b2t.b, writes=[b2t.b])
                      kb.dma("sp", wr[:], w_router[l * D:(l + 1) * D, :].rearrange("(kc p) n -> p kc n", p=128), wr.b, writes=[wr.b])
                      kb.dma("sp", brt[:], b_router[l:l + 1, :].partition_broadcast(128), brt.b, writes=[brt.b])
                      kb.op("pool", lambda e: e.iota(sel[:], [[1, E], [0, 128]], base=0, channel_multiplier=-1,
                                                     allow_small_or_imprecise_dtypes=True), writes=[sel.b])
                      kb.op("dve", lambda e: e.tensor_scalar(sel[:], sel[:], 0.0, None, ALU.is_equal), reads=[sel.b], writes=[sel.b])

                      gbs = [sb(ph, "gbs%d" % i, [128, ST]) for i in range(2)]
                      nsl = S // SL

                      def issue_w1(k):
                          if k >= nsl * n_exp * 4:
                              return
                          ex_i = (k // 4) % n_exp
                          q = k % 4
                          wp = w1p[k % 3]
                          w1v = w1[(l * E + ex_i) * D:(l * E + ex_i + 1) * D, :].rearrange("(kc p) n -> p kc n", p=128)
                          kb.dma("pool", wp[:], w1v[:, :, q * 512:(q + 1) * 512], wp.b, writes=[wp.b])

                      def issue_w2(m):
                          if m >= nsl * n_exp:
                              return
                          ex_i = m % n_exp
                          w2t = w2e[m % 2]
                          w2v = w2[(l * E + ex_i) * D:(l * E + ex_i + 1) * D, :].rearrange("(kc p) n -> p kc n", p=128)
                          kb.dma("pool", w2t[:], w2v, w2t.b, writes=[w2t.b])

                      issue_w1(0)
                      issue_w1(1)
                      issue_w2(0)
                      for sl in range(S // SL):
                          for half in range(2):
                              t0 = sl * SL + half * ST
                              s = t0 // ST
                              kb.dma("sp", hb[:], hT[:, t0:t0 + ST].rearrange("(kc p) t -> p kc t", p=128), hb.b,
                                     reads=[d_hT[s]], writes=[hb.b])
                              rmsnorm_to((sqc, rstdc), hb, 8, 24, fT, a_f32=None, a_off=half * ST)
                              for kc in range(KC):
                                  kb.op("act", lambda e, kc=kc: e.activation(sqc[:, kc, :], sqc[:, kc, :], AF.Identity,
                                                                              bias=modv[:, 24 + kc:25 + kc], scale=wv[:, 8 + kc:9 + kc]),
                                        reads=[sqc.b, modv.b, wv.b], writes=[sqc.b])
                              for tt in range(4):
                                  pl = PS[tt % 2]
                                  for kc in range(KC):
                                      kb.op("pe", lambda e, kc=kc, pl=pl, tt=tt: e.matmul(pl[:, 0:E], sqc[:, kc, tt * 128:(tt + 1) * 128], wr[:, kc, :],
                                                                                           start=(kc == 0), stop=(kc == KC - 1)),
                                            reads=[sqc.b, wr.b], writes=[pl.b])
                                  kb.op("dve", lambda e, pl=pl: e.tensor_tensor(lg[:], pl[:, 0:E], brt[:], ALU.add),
                                        reads=[pl.b, brt.b], writes=[lg.b])
                                  kb.op("dve", lambda e: e.max(mx8[:], lg[:]), reads=[lg.b], writes=[mx8.b])
                                  kb.op("dve", lambda e: e.tensor_scalar(mk[:], lg[:], mx8[:, 3:4], None, ALU.is_ge),
                                        reads=[lg.b, mx8.b], writes=[mk.b])
                                  kb.op("dve", lambda e: e.tensor_scalar(sm[:, 0:1], mx8[:, 0:1], -1.0, None, ALU.mult),
                                        reads=[mx8.b], writes=[sm.b])
                                  kb.op("act", lambda e: e.activation(ex[:], lg[:], AF.Exp, bias=sm[:, 0:1]),
                                        reads=[lg.b, sm.b], writes=[ex.b])
                                  kb.op("dve", lambda e: e.tensor_tensor(ex[:], ex[:], mk[:], ALU.mult), reads=[ex.b, mk.b], writes=[ex.b])
                                  kb.op("dve", lambda e: e.reduce_sum(sm[:, 1:2], ex[:], AX.X), reads=[ex.b], writes=[sm.b])
                                  kb.op("dve", lambda e: e.reciprocal(sm[:, 1:2], sm[:, 1:2]), reads=[sm.b], writes=[sm.b])
                                  kb.op("dve", lambda e: e.tensor_scalar(ex[:], ex[:], sm[:, 1:2], None, ALU.mult),
                                        reads=[ex.b, sm.b], writes=[ex.b])
                                  pt = PS[2 + tt % 2]
                                  kb.op("pe", lambda e, pt=pt: e.transpose(pt[0:32, 0:128], ex[:], ident[:]),
                                        reads=[ex.b, ident.b], writes=[pt.b])
                                  c0 = half * ST + tt * 128
                                  kb.op("act", lambda e, pt=pt, c0=c0: e.copy(GT[:, c0:c0 + 128], pt[0:32, 0:128]),
                                        reads=[pt.b], writes=[GT.b])
                          kb.op("dve", lambda e: e.tensor_copy(GTh[:], GT[:]), reads=[GT.b], writes=[GTh.b])
                          kb.op("dve", lambda e: e.tensor_copy(GTf[:], GTh[:]), reads=[GTh.b], writes=[GTf.b])
                          kb.op("dve", lambda e: e.tensor_tensor(GTf[:], GT[:], GTf[:], ALU.subtract), reads=[GT.b, GTf.b], writes=[GTf.b])
                          kb.op("dve", lambda e: e.tensor_copy(GTl[:], GTf[:]), reads=[GTf.b], writes=[GTl.b])
                          for half in range(2):
                              for dc in range(KC):
                                  p = PS[4 + dc % 2]
                                  kb.op("pe", lambda e, p=p, dc=dc, half=half: e.matmul(p[:], b2t[:, dc * 128:(dc + 1) * 128],
                                                                                         GT[:, half * ST:(half + 1) * ST], start=True, stop=True),
                                        reads=[b2t.b, GT.b], writes=[p.b])
                                  kb.op("act", lambda e, p=p, dc=dc, half=half: e.copy(yacc[:, dc, half * ST:(half + 1) * ST], p[:]),
                                        reads=[p.b], writes=[yacc.b])
                          for ex_i in range(n_exp):
                              m_idx = sl * n_exp + ex_i
                              w2t = w2e[m_idx % 2]
                              issue_w2(m_idx + 1)
                              for q in range(4):
                                  k_idx = m_idx * 4 + q
                                  wp = w1p[k_idx % 3]
                                  issue_w1(k_idx + 2)
                                  for half in range(2):
                                      hs = slice(half * ST, (half + 1) * ST)
                                      if q == 0:
                                          pg_ = PS[6 + half]
                                          kb.op("pe", lambda e, pg_=pg_, ex_i=ex_i, hs=hs: e.matmul(
                                              pg_[:], sel[:, ex_i * 128:(ex_i + 1) * 128], GTh[:, hs], start=True, stop=False),
                                              reads=[sel.b, GTh.b], writes=[pg_.b])
                                          kb.op("pe", lambda e, pg_=pg_, ex_i=ex_i, hs=hs: e.matmul(
                                              pg_[:], sel[:, ex_i * 128:(ex_i + 1) * 128], GTl[:, hs], start=False, stop=True),
                                              reads=[sel.b, GTl.b], writes=[pg_.b])
                                          kb.op("act", lambda e, pg_=pg_, half=half: e.copy(gbs[half][:], pg_[:]),
                                                reads=[pg_.b], writes=[gbs[half].b])
                                      for jl in range(2):
                                          jc = q * 2 + jl
                                          pgl = PS[(jl * 2) % 4]
                                          pll = PS[(jl * 2 + 1) % 4]
                                          for kc in range(KC):
                                              kb.op("pe", lambda e, kc=kc, pgl=pgl, wp=wp, jl=jl, hs=hs: e.matmul(
                                                  pgl[:], wp[:, kc, jl * 256:(jl + 1) * 256:2], fT[:, kc, hs],
                                                  start=(kc == 0), stop=(kc == KC - 1)),
                                                  reads=[wp.b, fT.b], writes=[pgl.b])
                                          for kc in range(KC):
                                              kb.op("pe", lambda e, kc=kc, pll=pll, wp=wp, jl=jl, hs=hs: e.matmul(
                                                  pll[:], wp[:, kc, jl * 256 + 1:(jl + 1) * 256:2], fT[:, kc, hs],
                                                  start=(kc == 0), stop=(kc == KC - 1)),
                                                  reads=[wp.b, fT.b], writes=[pll.b])
                                          bcol = ex_i * 16 + jc * 2
                                          x_g = xg[jl]
                                          s_g = sgm[jl]
                                          x_l = xl[jl]
                                          a_t = actT[half]
                                          g_b = gbs[half]
                                          kb.op("act", lambda e, pgl=pgl, x_g=x_g, bcol=bcol: e.activation(
                                              x_g[:], pgl[:], AF.Identity, bias=b1t[:, bcol:bcol + 1]),
                                              reads=[pgl.b, b1t.b], writes=[x_g.b])
                                          kb.op("act", lambda e, pll=pll, x_l=x_l, bcol=bcol: e.activation(
                                              x_l[:], pll[:], AF.Identity, bias=b1t[:, bcol + 1:bcol + 2]),
                                              reads=[pll.b, b1t.b], writes=[x_l.b])
                                          kb.op("pool", lambda e, x_g=x_g: e.tensor_scalar(x_g[:], x_g[:], 7.0, -1.0e30, ALU.min, ALU.max),
                                                reads=[x_g.b], writes=[x_g.b])
                                          kb.op("act", lambda e, x_g=x_g, s_g=s_g: e.activation(s_g[:], x_g[:], AF.Sigmoid, scale=1.702),
                                                reads=[x_g.b], writes=[s_g.b])
                                          kb.op("pool", lambda e, x_l=x_l: e.tensor_scalar(x_l[:], x_l[:], 7.0, -7.0, ALU.min, ALU.max),
                                                reads=[x_l.b], writes=[x_l.b])
                                          kb.op("pool", lambda e, s_g=s_g, g_b=g_b: e.tensor_tensor(s_g[:], s_g[:], g_b[:], ALU.mult),
                                                reads=[s_g.b, g_b.b], writes=[s_g.b])
                                          kb.op("dve", lambda e, x_g=x_g, s_g=s_g: e.tensor_tensor(x_g[:], x_g[:], s_g[:], ALU.mult),
                                                reads=[x_g.b, s_g.b], writes=[x_g.b])
                                          kb.op("dve", lambda e, x_g=x_g, x_l=x_l, a_t=a_t, jc=jc: e.scalar_tensor_tensor(
                                              a_t[:, jc, :], x_l[:], 1.0, x_g[:], ALU.add, ALU.mult),
                                              reads=[x_g.b, x_l.b], writes=[a_t.b])
                              for half in range(2):
                                  hs = slice(half * ST, (half + 1) * ST)
                                  a_t = actT[half]
                                  for dc in range(KC):
                                      p = PS[4 + dc % 2]
                                      for jc in range(KC):
                                          kb.op("pe", lambda e, jc=jc, p=p, dc=dc, a_t=a_t, w2t=w2t: e.matmul(
                                              p[:], w2t[:, jc, dc * 128:(dc + 1) * 128], a_t[:, jc, :],
                                              start=(jc == 0), stop=(jc == KC - 1)),
                                              reads=[w2t.b, a_t.b], writes=[p.b])
                                      kb.op("dve", lambda e, p=p, dc=dc, hs=hs: e.tensor_tensor(yacc[:, dc, hs], p[:], yacc[:, dc, hs], ALU.add),
                                            reads=[p.b, yacc.b], writes=[yacc.b])
                          for half in range(2):
                              t0 = sl * SL + half * ST
                              s = t0 // ST
                              kb.dma("sp", hb[:], hT[:, t0:t0 + ST].rearrange("(kc p) t -> p kc t", p=128), hb.b,
                                     reads=[d_hT[s]], writes=[hb.b])
                              for dc in range(KC):
                                  kb.op("dve", lambda e, dc=dc, half=half: e.scalar_tensor_tensor(
                                      hb[:, dc, :], yacc[:, dc, half * ST:(half + 1) * ST], modv[:, 40 + dc:41 + dc], hb[:, dc, :], ALU.mult, ALU.add),
                                      reads=[yacc.b, modv.b, hb.b], writes=[hb.b])
                              kb.dma("sp", hT[:, t0:t0 + ST].rearrange("(kc p) t -> p kc t", p=128), hb[:], hb.b,
                                     reads=[hb.b], writes=[d_hT[s]])
                      kb.barrier()

        for l in range(n_layers):
            one_layer(l)
            if stopf[0] or (tap is not None and l == cfg.get("tap_layer", 0)):
                break

        if tap is None:
            with ExitStack() as ph:
                hb = [sb(ph, "fhb%d" % i, [128, KC, ST]) for i in range(2)]
                sq = sb(ph, "fsq", [128, KC, ST])
                rstd = sb(ph, "frstd", [128, ST])
                fgt = sb(ph, "fgt", [128, KC])
                kb.dma("sp", fgt[:], fg, fgt.b, writes=[fgt.b])
                for s in range(NST):
                    t0 = s * ST
                    h_ = hb[s % 2]
                    ps = PS[s % 2]
                    kb.dma("sp", h_[:], (hT if n_layers > 0 else xT)[:, t0:t0 + ST].rearrange("(kc p) t -> p kc t", p=128), h_.b,
                           reads=[d_hT[s]], writes=[h_.b])
                    kb.op("act", lambda e, h_=h_: e.activation(sq[:], h_[:], AF.Square), reads=[h_.b], writes=[sq.b])
                    for kc in range(KC):
                        kb.op("pe", lambda e, kc=kc, ps=ps: e.matmul(ps[:], ones_f[:], sq[:, kc, :], start=(kc == 0), stop=(kc == KC - 1)),
                              reads=[ones_f.b, sq.b], writes=[ps.b])
                    kb.op("act", lambda e, ps=ps: e.activation(rstd[:], ps[:], AF.Sqrt, bias=eps_t[:, 0:1], scale=1.0 / D),
                          reads=[ps.b, eps_t.b], writes=[rstd.b])
                    kb.op("dve", lambda e: e.reciprocal(rstd[:], rstd[:]), reads=[rstd.b], writes=[rstd.b])
                    for kc in range(KC):
                        kb.op("dve", lambda e, kc=kc, h_=h_: e.scalar_tensor_tensor(h_[:, kc, :], h_[:, kc, :], fgt[:, kc:kc + 1], rstd[:],
                                                                                     ALU.mult, ALU.mult),
                              reads=[h_.b, fgt.b, rstd.b], writes=[h_.b])
                    kb.dma("sp", outT[:, t0:t0 + ST].rearrange("(kc p) t -> p kc t", p=128), h_[:], h_.b,
                           reads=[h_.b], writes=[d_out])
                kb.barrier()
        else:
            with ExitStack() as ph:
                z = sb(ph, "ztile", [128, 512])
                kb.dma("sp", z[:], xT[0:128, 0:512], z.b, writes=[z.b])
                kb.dma("sp", outT[0:128, 0:512], z[:], z.b, reads=[z.b], writes=[d_out])
                kb.barrier()

        kb.emit()
    return nc


def _swap_perm():
    idx = np.arange(1536)
    d = idx % 64
    return (idx - d) + (d + 32) % 64


def make_in_maps(inputs, ncores=NCORES):
    f = lambda a: np.ascontiguousarray(np.asarray(a, dtype=np.float32))
    x = np.asarray(inputs["x"], dtype=np.float32)
    c = np.asarray(inputs["c"], dtype=np.float32)
    pos = np.asarray(inputs["positions"]).astype(np.int32)
    w_in = f(inputs["w_in"])
    half = 32
    inv_freq = (np.float32(10000.0) ** (-np.arange(half, dtype=np.float32) / np.float32(half))).astype(np.float32)
    cst = np.zeros((128, 4), np.float32)
    p = np.arange(128)
    cst[:, 0] = inv_freq[p % 32]
    cst[:, 1] = np.where((p % 64) < 32, -1.0, 1.0)
    tri = (np.arange(128)[:, None] <= np.arange(128)[None, :]).astype(np.float32)
    maskT = np.tile(tri, (1, 4))
    kq = np.arange(128)
    prev = (kq[None, :] <= kq[:, None]).astype(np.float32)
    cur = (kq[:, None] <= kq[None, :]).astype(np.float32)
    attm = np.concatenate([prev, prev, cur, cur], axis=1)
    perm = _swap_perm()
    shared = {
        "cst": cst,
        "w_ada": f(inputs["w_ada"]).reshape(DEPTH * D, 6 * D),
        "b_adaT": f(np.asarray(inputs["b_ada"], np.float32).reshape(DEPTH, 48, 128).transpose(0, 2, 1)).reshape(DEPTH * 128, 48),
        "n1g": f(np.asarray(inputs["norm1_g"], np.float32).reshape(DEPTH, KC, 128).transpose(0, 2, 1)).reshape(DEPTH * 128, KC),
        "n2g": f(np.asarray(inputs["norm2_g"], np.float32).reshape(DEPTH, KC, 128).transpose(0, 2, 1)).reshape(DEPTH * 128, KC),
        "fg": f(np.asarray(inputs["final_g"], np.float32).reshape(KC, 128).T),
        "w_in": w_in.reshape(DEPTH * D, INW),
        "w_in_sw": f(w_in[:, :, :1536][:, :, perm]).reshape(DEPTH * D, 1536),
        "w_sT": f(np.asarray(inputs["w_s"], np.float32).transpose(0, 3, 1, 2)).reshape(DEPTH * 128, 512),
        "maskT": maskT,
        "attm": attm,
        "b_s": f(inputs["b_s"]).reshape(DEPTH, 512),
        "ln_g": f(inputs["ln_g"]),
        "ln_b": f(inputs["ln_b"]),
        "w_pa": f(inputs["w_pa"]).reshape(DEPTH * 256, D),
        "w_pg": f(inputs["w_pg"]).reshape(DEPTH * 512, D),
        "w_o": f(inputs["w_o"]).reshape(DEPTH * D, D),
        "w_router": f(inputs["w_router"]).reshape(DEPTH * D, E),
        "b_router": f(inputs["b_router"]),
        "w1": f(inputs["w1"]).reshape(DEPTH * E * D, 2 * D),
        "b1T": f(np.asarray(inputs["b1"], np.float32).reshape(DEPTH, E, 8, 128, 2).transpose(0, 3, 1, 2, 4)).reshape(DEPTH * 128, E * 16),
        "w2": f(inputs["w2"]).reshape(DEPTH * E * D, D),
        "b2": f(inputs["b2"]).reshape(DEPTH * E, D),
    }
    maps = []
    for b in range(ncores):
        m = dict(shared)
        m["xT"] = f(x[b].T)
        m["cT"] = f(c[b].reshape(KC, 128).T)
        m["pos"] = np.ascontiguousarray(pos[b].reshape(1, S))
        maps.append(m)
    return maps


def kernel(**inputs):
    nc = build_nc()
    maps = make_in_maps(inputs)
    res = run_bass_kernel_spmd(nc, maps, core_ids=list(range(NCORES)))
    out = np.stack([np.ascontiguousarray(r["outT"].T) for r in res.results], axis=0)
    return out.astype(np.float32)
```
